# Optimizing a Trainium2 kernel written in Bass

```python
import math
import jax, jax.numpy as jnp
from jax import lax
import numpy as np

D_MODEL = 1024
BATCH = 16
SEQ = 256
DEPTH = 2
DEC_BATCH = 2
DEC_SEQ = 1024
PAST_LEN = 512

GRID_W = 64
EPS = 1e-6
CONV_W = 512
CONV_K = 3
LRU_W = 1024
LRU_BLOCKS = 8
LRU_BW = LRU_W // LRU_BLOCKS
LRU_CONV_K = 4
LRU_C = 8.0
MLA_HEADS = 8
Q_RANK = 384
KV_RANK = 256
NOPE_DIM = 128
ROPE_DIM = 64
V_DIM = 128
QK_DIM = NOPE_DIM + ROPE_DIM
ROPE_THETA = 10000.0
Q_BLOCK = 128
HY_W = 512
HY_SHORT_K = 3
HY_BANDS = 16
HY_EMB = 1 + 2 * HY_BANDS
HY_HIDDEN = 64
HY_FAST_DECAY = 0.3
HY_SLOW_DECAY = 1.5
HY_TARGET = 1e-2
D_FF = 2816
N_EXPERTS = 8
TOP_K = 2
D_FF_EXPERT = 1408
IN0 = 3 * CONV_W + 2 * LRU_W
MIX0 = CONV_W + LRU_W
IN1 = Q_RANK + KV_RANK + ROPE_DIM + 3 * HY_W
MIX1 = MLA_HEADS * V_DIM + HY_W

kernel_name = 'hybrid_diffusion_prefix_trunk_step'

F32 = jnp.float32


def rmsnorm(x, g):
    xf = x.astype(F32)
    y = xf * lax.rsqrt(jnp.mean(xf * xf, axis=-1, keepdims=True) + EPS)
    return (y * g.astype(F32)).astype(x.dtype)


def adaln(cond, w_mod, b_mod):
    m = jax.nn.silu(cond) @ w_mod + b_mod
    return jnp.split(m, 6, axis=-1)


def modulate(x, shift, scale):
    return x * (1.0 + scale[:, None, :]) + shift[:, None, :]


def dwconv(x, w, pad_left, pad_right):
    ch = x.shape[-1]
    return lax.conv_general_dilated(
        x, w[:, None, :].astype(x.dtype), window_strides=(1,),
        padding=[(pad_left, pad_right)], dimension_numbers=('NWC', 'WIO', 'NWC'),
        feature_group_count=ch)


def linear_scan(a, b, h0, reverse):
    if h0 is not None:
        idx = -1 if reverse else 0
        b = b.at[:, idx].add(a[:, idx] * h0)

    def comb(left, right):
        al, bl = left
        ar, br = right
        return al * ar, ar * bl + br

    _, h = lax.associative_scan(comb, (a, b), axis=1, reverse=reverse)
    return h


def rglru_dir(x, wa, ba, wi, bi, lam, h0, reverse):
    bsz, L, _ = x.shape
    xb = x.reshape(bsz, L, LRU_BLOCKS, LRU_BW)
    r = jax.nn.sigmoid(jnp.einsum('blnh,nhk->blnk', xb, wa).reshape(bsz, L, LRU_W) + ba)
    i = jax.nn.sigmoid(jnp.einsum('blnh,nhk->blnk', xb, wi).reshape(bsz, L, LRU_W) + bi)
    log_a = -LRU_C * r * jax.nn.softplus(-lam.astype(F32))
    a = jnp.exp(log_a)
    mult = jnp.sqrt(-jnp.expm1(2.0 * log_a))
    return linear_scan(a, mult * (i * x), h0, reverse)


def even_mixer(h, h0, w_in, conv_a, lru_conv_w, lru_conv_b, wa, ba, wi, bi, lam, w_out):
    u = h @ w_in
    ua, ub = u[..., :3 * CONV_W], u[..., 3 * CONV_W:]
    b_g, c_g, x_a = jnp.split(ua, 3, axis=-1)
    y_a = b_g * dwconv(c_g * x_a, conv_a, 1, 1)
    gate, x_b = jnp.split(ub, 2, axis=-1)
    xc = (dwconv(x_b, lru_conv_w, 2, 1) + lru_conv_b).astype(F32)
    h0f = None if h0 is None else h0[:, 0].astype(F32)
    h0b = None if h0 is None else h0[:, 1].astype(F32)
    hf = rglru_dir(xc, wa[0], ba[0], wi[0], bi[0], lam[0], h0f, False)
    hb = rglru_dir(xc, wa[1], ba[1], wi[1], bi[1], lam[1], h0b, True)
    y_b = (hf + hb).astype(h.dtype) * jax.nn.gelu(gate)
    y = jnp.concatenate([y_a, y_b], axis=-1) @ w_out
    if h0 is None:
        return y, jnp.stack([hf[:, -1], hb[:, 0]], axis=1).astype(h.dtype)
    return y, None


def axial_rope(x):
    L = x.shape[1]
    rows = L // GRID_W
    row = jnp.repeat(jnp.arange(rows, dtype=F32), GRID_W)
    col = jnp.tile(jnp.arange(GRID_W, dtype=F32), rows)
    half = ROPE_DIM // 2
    n_freq = half // 2
    inv = ROPE_THETA ** (-jnp.arange(n_freq, dtype=F32) / n_freq)
    ang = jnp.concatenate([row[:, None] * inv, col[:, None] * inv], axis=-1)
    shape = (L,) + (1,) * (x.ndim - 3) + (half,)
    cos, sin = jnp.cos(ang).reshape(shape), jnp.sin(ang).reshape(shape)
    xf = x.astype(F32)
    x1, x2 = xf[..., :half], xf[..., half:]
    return jnp.concatenate([x1 * cos - x2 * sin, x1 * sin + x2 * cos], axis=-1).astype(x.dtype)


def attend(q, k, v):
    bsz, lq, nh, dk = q.shape
    nb = lq // Q_BLOCK
    scale = 1.0 / math.sqrt(dk)
    qb = q.reshape(bsz, nb, Q_BLOCK, nh, dk).transpose(1, 0, 2, 3, 4)

    def blk(qi):
        s = jnp.einsum('bqhd,bkhd->bhqk', qi, k).astype(F32) * scale
        p = jax.nn.softmax(s, axis=-1)
        return jnp.einsum('bhqk,bkhd->bqhd', p.astype(v.dtype), v)

    o = lax.map(blk, qb)
    return o.transpose(1, 0, 2, 3, 4).reshape(bsz, lq, nh * v.shape[-1])


def mla_kv(ckv, krope, w_kv_up):
    bsz, L, _ = ckv.shape
    kv = (ckv @ w_kv_up).reshape(bsz, L, MLA_HEADS, NOPE_DIM + V_DIM)
    k_nope, v = kv[..., :NOPE_DIM], kv[..., NOPE_DIM:]
    k_pe = jnp.broadcast_to(krope[:, :, None, :], (bsz, L, MLA_HEADS, ROPE_DIM))
    return jnp.concatenate([k_nope, k_pe.astype(k_nope.dtype)], axis=-1), v


def hyena_filter(L, w1, b1, w2, b2, w3):
    t = jnp.linspace(0.0, 1.0, L, dtype=F32)[:, None]
    w = 2.0 * math.pi * jnp.arange(L, dtype=F32)[:, None] / L
    f = jnp.linspace(1e-4, HY_BANDS - 1, HY_BANDS, dtype=F32)[None, :]
    z = jnp.concatenate([t, jnp.cos(f * w), -jnp.sin(f * w)], axis=-1)
    hid = jnp.sin(z @ w1.astype(F32) + b1.astype(F32))
    hid = jnp.sin(hid @ w2.astype(F32) + b2.astype(F32))
    hf = hid @ w3.astype(F32)
    max_decay = math.log(HY_TARGET) / HY_FAST_DECAY
    min_decay = math.log(HY_TARGET) / HY_SLOW_DECAY
    deltas = jnp.linspace(min_decay, max_decay, HY_W, dtype=F32)
    decay = jnp.exp(-t * jnp.abs(deltas))
    h_fwd = hf[:, :HY_W] * decay
    h_bwd = hf[:, HY_W:] * decay
    k = jnp.concatenate([h_fwd, jnp.zeros((1, HY_W), F32), jnp.flip(h_bwd[1:], axis=0)], axis=0)
    return k / jnp.sum(jnp.abs(k), axis=0, keepdims=True)


def long_conv(u, k, bias):
    L = u.shape[1]
    uf = jnp.fft.rfft(u.astype(F32), n=2 * L, axis=1)
    kf = jnp.fft.rfft(k, n=2 * L, axis=0)
    y = jnp.fft.irfft(uf * kf[None], n=2 * L, axis=1)[:, :L]
    return (y + u.astype(F32) * bias.astype(F32)).astype(u.dtype)


def odd_mixer(h, ctx, w_in, q_norm, kv_norm, w_q_up, w_kv_up, hy_short_w, hy_short_b,
              f_w1, f_b1, f_w2, f_b2, f_w3, hy_bias, w_out):
    bsz, L, _ = h.shape
    u = h @ w_in
    o1, o2, o3 = Q_RANK, Q_RANK + KV_RANK, Q_RANK + KV_RANK + ROPE_DIM
    cq, ckv_raw, kr, uh = u[..., :o1], u[..., o1:o2], u[..., o2:o3], u[..., o3:]
    q = (rmsnorm(cq, q_norm) @ w_q_up).reshape(bsz, L, MLA_HEADS, QK_DIM)
    ckv = rmsnorm(ckv_raw, kv_norm)
    if ctx is None:
        k, v = mla_kv(ckv, kr, w_kv_up)
        cache = (ckv, kr)
    else:
        q = jnp.concatenate([q[..., :NOPE_DIM], axial_rope(q[..., NOPE_DIM:])], axis=-1)
        k_l, v_l = mla_kv(ckv, axial_rope(kr), w_kv_up)
        k_c, v_c = mla_kv(ctx[0].astype(h.dtype), ctx[1].astype(h.dtype), w_kv_up)
        k = jnp.concatenate([k_c, k_l], axis=1)
        v = jnp.concatenate([v_c, v_l], axis=1)
        cache = None
    y_c = attend(q, k, v)
    uc = dwconv(uh, hy_short_w, 1, 1) + hy_short_b
    x0, x1, vv = jnp.split(uc, 3, axis=-1)
    kfilt = hyena_filter(L, f_w1, f_b1, f_w2, f_b2, f_w3)
    y_d = x0 * long_conv(x1 * vv, kfilt, hy_bias)
    y = jnp.concatenate([y_c, y_d], axis=-1) @ w_out
    return y, cache


def swiglu(h, w_gate, w_up, w_down):
    return (jax.nn.silu(h @ w_gate) * (h @ w_up)) @ w_down


def moe_swiglu(h, w_router, b_router, e_gate, e_up, e_down):
    bsz, L, d = h.shape
    ht = h.reshape(-1, d)
    logits = (ht @ w_router).astype(F32) + b_router.astype(F32)
    probs = jax.nn.softmax(logits, axis=-1)
    top_p, top_i = lax.top_k(probs, TOP_K)
    top_p = top_p / jnp.sum(top_p, axis=-1, keepdims=True)
    gates = jnp.sum(jax.nn.one_hot(top_i, N_EXPERTS, dtype=F32) * top_p[..., None], axis=1)
    hg = jnp.einsum('td,edf->tef', ht, e_gate)
    hu = jnp.einsum('td,edf->tef', ht, e_up)
    act = jax.nn.silu(hg) * hu * gates[..., None].astype(ht.dtype)
    out = jnp.einsum('tef,efd->td', act, e_down)
    return out.reshape(bsz, L, d)


def setup_inputs(seed: int = 0) -> dict:
    key = jax.random.key(seed)
    ks = iter(jax.random.split(key, 64))
    D = D_MODEL

    def nrm(shape, scale):
        return jax.random.normal(next(ks), shape, F32) * scale

    def gain(n):
        return 1.0 + nrm((n,), 0.1)

    inp = {}
    inp['x_prompt'] = nrm((BATCH, SEQ, D), 1.0)
    inp['x_sample'] = nrm((DEC_BATCH, DEC_SEQ, D), 1.0)
    inp['state_l0_lru'] = nrm((DEC_BATCH, 2, LRU_W), 0.5)
    inp['cache_l1_ckv'] = nrm((DEC_BATCH, PAST_LEN, KV_RANK), 1.0)
    inp['cache_l1_krope'] = nrm((DEC_BATCH, PAST_LEN, ROPE_DIM), 1.0)
    inp['c'] = nrm((DEC_BATCH, D), 1.0)
    inp['c_ctx'] = nrm((D,), 1.0)
    inp['l0_norm1'] = gain(D)
    inp['l0_norm2'] = gain(D)
    inp['l0_w_mod'] = nrm((D, 6 * D), 0.5 * D ** -0.5)
    inp['l0_b_mod'] = nrm((6 * D,), 0.02)
    inp['l0_w_in'] = nrm((D, IN0), D ** -0.5)
    inp['l0_conv_a'] = nrm((CONV_K, CONV_W), CONV_K ** -0.5)
    inp['l0_lru_conv_w'] = nrm((LRU_CONV_K, LRU_W), LRU_CONV_K ** -0.5)
    inp['l0_lru_conv_b'] = nrm((LRU_W,), 0.02)
    inp['l0_lru_wa'] = nrm((2, LRU_BLOCKS, LRU_BW, LRU_BW), LRU_BW ** -0.5)
    inp['l0_lru_ba'] = nrm((2, LRU_W), 0.02)
    inp['l0_lru_wi'] = nrm((2, LRU_BLOCKS, LRU_BW, LRU_BW), LRU_BW ** -0.5)
    inp['l0_lru_bi'] = nrm((2, LRU_W), 0.02)
    a0 = jax.random.uniform(next(ks), (2, LRU_W), F32, 0.9, 0.999)
    inp['l0_lru_lambda'] = jnp.log(a0) - jnp.log1p(-a0)
    inp['l0_w_out'] = nrm((MIX0, D), MIX0 ** -0.5)
    inp['l0_ffn_gate'] = nrm((D, D_FF), D ** -0.5)
    inp['l0_ffn_up'] = nrm((D, D_FF), D ** -0.5)
    inp['l0_ffn_down'] = nrm((D_FF, D), D_FF ** -0.5)
    inp['l1_norm1'] = gain(D)
    inp['l1_norm2'] = gain(D)
    inp['l1_w_mod'] = nrm((D, 6 * D), 0.5 * D ** -0.5)
    inp['l1_b_mod'] = nrm((6 * D,), 0.02)
    inp['l1_w_in'] = nrm((D, IN1), D ** -0.5)
    inp['l1_q_norm'] = gain(Q_RANK)
    inp['l1_kv_norm'] = gain(KV_RANK)
    inp['l1_w_q_up'] = nrm((Q_RANK, MLA_HEADS * QK_DIM), Q_RANK ** -0.5)
    inp['l1_w_kv_up'] = nrm((KV_RANK, MLA_HEADS * (NOPE_DIM + V_DIM)), KV_RANK ** -0.5)
    inp['l1_hy_short_w'] = nrm((HY_SHORT_K, 3 * HY_W), HY_SHORT_K ** -0.5)
    inp['l1_hy_short_b'] = nrm((3 * HY_W,), 0.02)
    inp['l1_hy_f_w1'] = nrm((HY_EMB, HY_HIDDEN), 1.0)
    inp['l1_hy_f_b1'] = nrm((HY_HIDDEN,), 0.1)
    inp['l1_hy_f_w2'] = nrm((HY_HIDDEN, HY_HIDDEN), HY_HIDDEN ** -0.5)
    inp['l1_hy_f_b2'] = nrm((HY_HIDDEN,), 0.1)
    inp['l1_hy_f_w3'] = nrm((HY_HIDDEN, 2 * HY_W), HY_HIDDEN ** -0.5)
    inp['l1_hy_bias'] = nrm((HY_W,), 1.0)
    inp['l1_w_out'] = nrm((MIX1, D), MIX1 ** -0.5)
    inp['l1_router_w'] = nrm((D, N_EXPERTS), D ** -0.5)
    inp['l1_router_b'] = nrm((N_EXPERTS,), 0.01)
    inp['l1_exp_gate'] = nrm((N_EXPERTS, D, D_FF_EXPERT), D ** -0.5)
    inp['l1_exp_up'] = nrm((N_EXPERTS, D, D_FF_EXPERT), D ** -0.5)
    inp['l1_exp_down'] = nrm((N_EXPERTS, D_FF_EXPERT, D), D_FF_EXPERT ** -0.5)
    inp['final_norm'] = gain(D)
    return inp


def reference(x_prompt, x_sample, state_l0_lru, cache_l1_ckv, cache_l1_krope, c, c_ctx,
              l0_norm1, l0_norm2, l0_w_mod, l0_b_mod, l0_w_in, l0_conv_a,
              l0_lru_conv_w, l0_lru_conv_b, l0_lru_wa, l0_lru_ba, l0_lru_wi, l0_lru_bi,
              l0_lru_lambda, l0_w_out, l0_ffn_gate, l0_ffn_up, l0_ffn_down,
              l1_norm1, l1_norm2, l1_w_mod, l1_b_mod, l1_w_in, l1_q_norm, l1_kv_norm,
              l1_w_q_up, l1_w_kv_up, l1_hy_short_w, l1_hy_short_b, l1_hy_f_w1, l1_hy_f_b1,
              l1_hy_f_w2, l1_hy_f_b2, l1_hy_f_w3, l1_hy_bias, l1_w_out, l1_router_w,
              l1_router_b, l1_exp_gate, l1_exp_up, l1_exp_down, final_norm):
    sub_params = ((l0_norm1, l0_norm2, l0_w_mod, l0_b_mod),
                  (l1_norm1, l1_norm2, l1_w_mod, l1_b_mod))
    mix_params = ((l0_w_in, l0_conv_a, l0_lru_conv_w, l0_lru_conv_b, l0_lru_wa, l0_lru_ba,
                   l0_lru_wi, l0_lru_bi, l0_lru_lambda, l0_w_out),
                  (l1_w_in, l1_q_norm, l1_kv_norm, l1_w_q_up, l1_w_kv_up, l1_hy_short_w,
                   l1_hy_short_b, l1_hy_f_w1, l1_hy_f_b1, l1_hy_f_w2, l1_hy_f_b2, l1_hy_f_w3,
                   l1_hy_bias, l1_w_out))
    ffn_params = ((l0_ffn_gate, l0_ffn_up, l0_ffn_down),
                  (l1_router_w, l1_router_b, l1_exp_gate, l1_exp_up, l1_exp_down))
    cached = (state_l0_lru, (cache_l1_ckv, cache_l1_krope))

    xp, xs = x_prompt, x_sample
    new_lru = new_ckv = new_krope = None
    for layer in range(DEPTH):
        n1, n2, w_mod, b_mod = sub_params[layer]
        pm = adaln(c_ctx[None, :], w_mod, b_mod)
        sm = adaln(c, w_mod, b_mod)
        hp = modulate(rmsnorm(xp, n1), pm[0], pm[1])
        hs = modulate(rmsnorm(xs, n1), sm[0], sm[1])
        if layer % 2 == 0:
            yp, new_lru = even_mixer(hp, None, *mix_params[layer])
            ys, _ = even_mixer(hs, cached[layer], *mix_params[layer])
        else:
            yp, ctx_cache = odd_mixer(hp, None, *mix_params[layer])
            new_ckv, new_krope = ctx_cache
            ys, _ = odd_mixer(hs, cached[layer], *mix_params[layer])
        xp = xp + pm[2][:, None, :] * yp
        xs = xs + sm[2][:, None, :] * ys
        hp = modulate(rmsnorm(xp, n2), pm[3], pm[4])
        hs = modulate(rmsnorm(xs, n2), sm[3], sm[4])
        if layer % 2 == 0:
            fp, fs = swiglu(hp, *ffn_params[layer]), swiglu(hs, *ffn_params[layer])
        else:
            fp, fs = moe_swiglu(hp, *ffn_params[layer]), moe_swiglu(hs, *ffn_params[layer])
        xp = xp + pm[5][:, None, :] * fp
        xs = xs + sm[5][:, None, :] * fs

    y_prompt = rmsnorm(xp, final_norm)
    y_sample = rmsnorm(xs, final_norm)
    return (y_prompt, y_sample, new_lru, new_ckv, new_krope)
```

```python
import contextlib
import math
import numpy as np
import ml_dtypes
import concourse.bass as bass
import concourse.mybir as mybir
from concourse.bass_utils import run_bass_kernel_spmd

F32 = mybir.dt.float32
BF16 = mybir.dt.bfloat16
I32 = mybir.dt.int32
ALU = mybir.AluOpType
AF = mybir.ActivationFunctionType

T = 1024
D = 1024
SEG = 256
NSEG = 4
EPS = 1e-6
NCORES = 8
LCTX = 512
LK = LCTX + T
NKT = LK // 128
NEG = -30000.0


class Tl:
    def __init__(self, t, name):
        self.t = t
        self.name = name
        self.w = []
        self.r = []
        self.dsem = None
        self.dcnt = 0
        self.psum = False
        self.persistent = False


class Eng:
    def __init__(self, h, sem, name, is_pe=False):
        self.h = h
        self.sem = sem
        self.name = name
        self.n = 0
        self.known = {}
        self.is_pe = is_pe
        self.pending = {}


class KB:
    def __init__(self, nc):
        self.nc = nc
        self.es = contextlib.ExitStack()
        self.sems = {}
        self.nsem = 0
        self.pe = self._eng(nc.tensor, "pe", True)
        self.act = self._eng(nc.scalar, "act")
        self.dve = self._eng(nc.vector, "dve")
        self.pool = self._eng(nc.gpsimd, "pool")
        self.sp = self._eng(nc.sync, "sp")
        self.engs = [self.pe, self.act, self.dve, self.pool, self.sp]
        self.dma_sems = []
        self.uid = 0
        self.banks = [Tl(self.es.enter_context(nc.psum_tensor(f"bank{i}", [128, 512], F32)), f"bank{i}")
                      for i in range(8)]
        for b in self.banks:
            b.psum = True
            b.persistent = True
        self.bank_i = 0
        self.reserved = []

    def _eng(self, h, name, is_pe=False):
        return Eng(h, self.newsem("e_" + name), name, is_pe)

    def newsem(self, name=None):
        self.nsem += 1
        name = name or f"s{self.nsem}"
        s = self.es.enter_context(self.nc.semaphore(name))
        self.sems[name] = s
        return name

    def sb(self, name, shape, dt=F32, stack=None):
        self.uid += 1
        st = stack if stack is not None else self.es
        t = st.enter_context(self.nc.sbuf_tensor(f"{name}_{self.uid}", list(shape), dt))
        tl = Tl(t, name)
        tl.persistent = stack is None
        return tl

    def bank(self):
        while True:
            b = self.banks[self.bank_i]
            self.bank_i = (self.bank_i + 1) % 8
            if b not in self.reserved:
                return b

    def reserve(self):
        b = self.bank()
        self.reserved.append(b)
        return b

    def release(self, b):
        self.reserved.remove(b)

    def _need(self, r, w):
        need = {}
        for t in r:
            for (s, v) in t.w:
                if need.get(s, 0) < v:
                    need[s] = v
            if t.psum:
                for (s, v) in t.r:
                    if need.get(s, 0) < v:
                        need[s] = v
        for t in w:
            for (s, v) in t.w:
                if need.get(s, 0) < v:
                    need[s] = v
            for (s, v) in t.r:
                if need.get(s, 0) < v:
                    need[s] = v
        return need

    def _waits(self, e, need):
        for s, v in need.items():
            if e.is_pe and s == e.sem:
                continue
            if e.known.get(s, 0) >= v:
                continue
            e.h.wait_ge(self.sems[s], v)
            e.known[s] = v

    @staticmethod
    def _addr(t, ev):
        for i, (s, v) in enumerate(t.r):
            if s == ev[0]:
                if v < ev[1]:
                    t.r[i] = ev
                return
        t.r.append(ev)

    def _flush(self, e):
        if e.pending:
            self._waits(e, e.pending)
            e.pending = {}

    def op(self, e, fn, r=(), w=(), inc=True):
        if any(not t.persistent for t in w):
            self._flush(e)
        self._waits(e, self._need(r, w))
        ins = fn(e.h)
        if inc:
            e.n += 1
            ins.then_inc(self.sems[e.sem], 1)
            ev = (e.sem, e.n)
        else:
            ev = (e.sem, e.n + 1)
        for t in w:
            t.w = [ev]
            t.r = []
        for t in r:
            self._addr(t, ev)
        return ins

    def mm(self, bank, out_ap, lhsT, rhs, r, start, stop, first=None, last=None, finc=False):
        first = start if first is None else first
        last = stop if last is None else last
        e = self.pe
        need = self._need(r, (bank,) if first else ())
        self._waits(e, need)
        ins = e.h.matmul(out_ap, lhsT, rhs, start=start, stop=stop)
        if first:
            bank.w = []
            bank.r = []
        if last or finc or stop:
            e.n += 1
            ins.then_inc(self.sems[e.sem], 1)
            ev = (e.sem, e.n)
            if last:
                bank.w = [ev]
        else:
            ev = (e.sem, e.n + 1)
        for t in r:
            self._addr(t, ev)

    def transpose(self, bank, out_ap, in_ap, ident_ap, r, first=True, last=True):
        e = self.pe
        self._waits(e, self._need(r, (bank,) if first else ()))
        ins = e.h.transpose(out_ap, in_ap, ident_ap)
        if first:
            bank.w = []
            bank.r = []
        if last:
            e.n += 1
            ins.then_inc(self.sems[e.sem], 1)
            ev = (e.sem, e.n)
            bank.w = [ev]
        else:
            ev = (e.sem, e.n + 1)
        for t in r:
            self._addr(t, ev)

    def dma(self, q, out, in_, st, r=(), w=(), lazy_ok=False):
        if any(not t.persistent for t in w):
            self._flush(q)
        self._waits(q, self._need(r, w))
        ins = q.h.dma_start(out=out, in_=in_)
        if st.dsem is None:
            st.dsem = {}
        if q.name not in st.dsem:
            st.dsem[q.name] = [self.newsem(), 0]
            self.dma_sems.append(st.dsem[q.name])
        ds = st.dsem[q.name]
        ds[1] += 16
        ins.then_inc(self.sems[ds[0]], 16)
        ev = (ds[0], ds[1])
        for t in w:
            t.w = [ev]
            t.r = []
        for t in r:
            self._addr(t, ev)

    def barrier(self):
        for e in self.engs:
            tgt = {}
            for f in self.engs:
                if f is e or f.n == 0:
                    continue
                tgt[f.sem] = f.n
            for ds in self.dma_sems:
                tgt[ds[0]] = ds[1]
            for s_, v in tgt.items():
                if e.pending.get(s_, 0) < v:
                    e.pending[s_] = v

    def finish(self):
        sp = self.sp
        for f in self.engs:
            if f is sp or f.n == 0:
                continue
            if sp.known.get(f.sem, 0) < f.n:
                sp.h.wait_ge(self.sems[f.sem], f.n)
        for ds in self.dma_sems:
            sp.h.wait_ge(self.sems[ds[0]], ds[1])
        self.es.close()


class Ring:
    def __init__(self, K, n):
        self.K = K
        self.slots = [K.sb(f"ring{i}", [128, 4096], BF16) for i in range(n)]
        self.i = 0

    def load(self, src3, k, n, q=None, hold=False):
        K = self.K
        while True:
            s = self.slots[self.i]
            self.i = (self.i + 1) % len(self.slots)
            if not getattr(s, "held", False):
                break
        s.held = hold
        dst = s.t[:, 0:k * n].rearrange("p (k n) -> p k n", k=k)
        K.dma(q or K.pool, dst, src3, s, w=(s,), lazy_ok=True)
        return s, dst


def wview(w, r0, k, c0, n):
    return w[r0:r0 + 128 * k, c0:c0 + n].rearrange("(k p) n -> p k n", p=128)


def build(stage=99, debug=False):
    nc = bass.Bass("TRN2", target_bir_lowering=False)
    K = KB(nc)
    pe, act, dve, pool, sp = K.pe, K.act, K.dve, K.pool, K.sp

    def din(name, shape, dt=F32):
        return nc.dram_tensor(name, list(shape), dt, kind="ExternalInput").ap()

    def dout(name, shape, dt=F32):
        return nc.dram_tensor(name, list(shape), dt, kind="ExternalOutput").ap()

    xT_d = din("xT", [D, T])
    cond_d = din("cond", [128, 8])
    h0_d = din("h0", [128, 16])
    pflag_d = din("flags", [128, 4])
    y_d = dout("yT", [D, T])
    lru_d = dout("lru_out", [128, 2 * 8 * 4])
    W = {}
    for L in (0, 1):
        W[f"l{L}_norm1"] = din(f"l{L}_norm1", [128, 8])
        W[f"l{L}_norm2"] = din(f"l{L}_norm2", [128, 8])
        W[f"l{L}_w_mod"] = din(f"l{L}_w_mod", [D, 6 * D])
        W[f"l{L}_b_mod"] = din(f"l{L}_b_mod", [128, 48])
    W["l0_w_in"] = din("l0_w_in", [D, 3584])
    W["l0_conv_a"] = din("l0_conv_a", [128, 12])
    W["l0_lru_conv_w"] = din("l0_lru_conv_w", [128, 32])
    W["l0_lru_conv_b"] = din("l0_lru_conv_b", [128, 8])
    W["l0_lru_wa"] = din("l0_lru_wa", [16 * 128, 128])
    W["l0_lru_wi"] = din("l0_lru_wi", [16 * 128, 128])
    W["l0_lru_ba"] = din("l0_lru_ba", [128, 16])
    W["l0_lru_bi"] = din("l0_lru_bi", [128, 16])
    W["l0_lru_lambda"] = din("l0_lru_lambda", [128, 16])
    W["l0_w_out"] = din("l0_w_out", [1536, D])
    W["l0_ffn_gate"] = din("l0_ffn_gate", [D, 2816])
    W["l0_ffn_up"] = din("l0_ffn_up", [D, 2816])
    W["l0_ffn_down"] = din("l0_ffn_down", [2816, D])
    W["final_norm"] = din("final_norm", [128, 8])
    W["l1_w_in"] = din("l1_w_in", [D, 2240])
    W["l1_w_in_krsw"] = din("l1_w_in_krsw", [D, 64])
    W["l1_q_norm"] = din("l1_q_norm", [128, 3])
    W["l1_kv_norm"] = din("l1_kv_norm", [128, 2])
    W["l1_wq_nope"] = din("l1_wq_nope", [384, 1024])
    W["l1_wq_rope"] = din("l1_wq_rope", [384, 512])
    W["l1_wq_rope_sw"] = din("l1_wq_rope_sw", [384, 512])
    W["l1_wk_nope"] = din("l1_wk_nope", [256, 1024])
    W["l1_wv"] = din("l1_wv", [256, 1024])
    W["l1_hy_short_w"] = din("l1_hy_short_w", [128, 36])
    W["l1_hy_short_b"] = din("l1_hy_short_b", [128, 12])
    W["l1_hy_f_w1"] = din("l1_hy_f_w1", [33, 64])
    W["l1_hy_f_b1"] = din("l1_hy_f_b1", [64, 1])
    W["l1_hy_f_w2"] = din("l1_hy_f_w2", [64, 64])
    W["l1_hy_f_b2"] = din("l1_hy_f_b2", [64, 1])
    W["l1_hy_f_w3"] = din("l1_hy_f_w3", [64, 1024])
    W["l1_hy_bias"] = din("l1_hy_bias", [128, 4])
    W["l1_w_out"] = din("l1_w_out", [1536, D])
    W["l1_router_w"] = din("l1_router_w", [D, 8])
    W["l1_router_b"] = din("l1_router_b", [8, 1])
    W["l1_exp_gate"] = din("l1_exp_gate", [8, D, 1408])
    W["l1_exp_up"] = din("l1_exp_up", [8, D, 1408])
    W["l1_exp_down"] = din("l1_exp_down", [8, 1408, D])
    sel8_d = din("sel8", [8, 1024], BF16)
    Fm_d = din("Fm", [T, 2 * T], BF16)
    iFm_d = din("iFm", [4, 2 * T, T], BF16)
    ctx_ckv_d = din("ctx_ckvT", [256, LCTX])
    ctx_kr_d = din("ctx_krT", [64, LCTX])
    ropecos_d = din("ropecos", [64, T])
    ropesin_d = din("ropesin", [64, T])
    mk_l_d = din("mk_l", [4, NKT * 128], BF16)
    mk_r_d = din("mk_r", [4, T], BF16)
    zin_d = din("zin", [33, T])
    decay_d = din("decay", [T, 512])
    ckv_out_d = dout("ckv_out", [256, T])
    kr_out_d = dout("kr_out", [64, T])
    dbg_d = dout("dbg", [8, 128, T]) if debug else None

    ring = Ring(K, 6)
    xs = K.sb("x", [128, 8, T], F32)
    xh = [[Tl(xs.t, f"x{c}_{hh}") for hh in range(2)] for c in range(8)]
    for row in xh:
        for t_ in row:
            t_.persistent = True
    hs = K.sb("h", [128, 8, T], BF16)
    hh_ = [[Tl(hs.t, f"h{c}_{hh}") for hh in range(2)] for c in range(8)]
    for row in hh_:
        for t_ in row:
            t_.persistent = True
    ym = [K.sb(f"ymix{c}", [128, T], BF16) for c in range(12)]
    ones_bf = K.sb("ones_bf", [128, 128], BF16)
    ident = K.sb("ident", [128, 128], F32)
    cond = K.sb("cond", [128, 8], F32)
    siluc = K.sb("siluc", [128, 8], BF16)
    flags = K.sb("flags", [128, 4], F32)
    fin_g = K.sb("fin_g", [128, 8], F32)
    dbg_tile = K.sb("dbgt", [128, T], F32) if debug else None

    def X(c, hh):
        return xs.t[:, c, hh * 512:(hh + 1) * 512]

    def H(c, hh):
        return hs.t[:, c, hh * 512:(hh + 1) * 512]

    def small(name, d_ap, shape, dt=F32, stack=None, q=None):
        t = K.sb(name, shape, dt, stack)
        K.dma(q or sp, t.t[:], d_ap, t, w=(t,))
        return t

    K.op(dve, lambda h: h.memset(ones_bf.t[:], 1.0), w=(ones_bf,))
    K.op(pool, lambda h: h.memset(ident.t[:], 1.0), w=(ident,))
    K.op(pool, lambda h: h.affine_select(out=ident.t[:], in_=ident.t[:], pattern=[[-1, 128]],
                                         compare_op=ALU.is_equal, fill=0.0, base=0, channel_multiplier=1),
         r=(ident,), w=(ident,))
    K.dma(sp, cond.t[:], cond_d, cond, w=(cond,))
    K.dma(sp, flags.t[:], pflag_d, flags, w=(flags,))
    K.dma(sp, fin_g.t[:], W["final_norm"], fin_g, w=(fin_g,))
    xv = xT_d.rearrange("(c p) t -> p c t", p=128)
    for c in range(8):
        K.dma(sp, xs.t[:, c, :], xv[:, c, :], xh[c][0], w=(xh[c][0], xh[c][1]))
    K.op(act, lambda h: h.activation(out=siluc.t[:], in_=cond.t[:], func=AF.Silu), r=(cond,), w=(siluc,))

    rot = {}

    def rotbuf(name, n, shape, dt, stack):
        if not hasattr(stack, "rot"):
            stack.rot = {}
        if name not in stack.rot:
            stack.rot[name] = [[K.sb(f"{name}{i}", shape, dt, stack) for i in range(n)], 0]
        lst = stack.rot[name]
        b = lst[0][lst[1] % n]
        lst[1] += 1
        return b

    bg_tasks = []

    def bg_step(n=1):
        for _ in range(n):
            if bg_tasks:
                bg_tasks.pop(0)()

    def zipper(gF, gB, nf=1, nb=1):
        aF, aB = gF is not None, gB is not None
        while aF or aB:
            for _ in range(nf):
                if aF:
                    try:
                        next(gF)
                    except StopIteration:
                        aF = False
            for _ in range(nb):
                if aB:
                    try:
                        next(gB)
                    except StopIteration:
                        aB = False
    def adaln_alloc(L, stack):
        t = {}
        t["bmod"] = small(f"bmod{L}", W[f"l{L}_b_mod"], [128, 48], stack=stack)
        t["mod"] = K.sb(f"mod{L}", [128, 48], F32, stack)
        t["moda"] = K.sb(f"moda{L}", [128, 16], F32, stack)
        t["n1"] = small(f"n1_{L}", W[f"l{L}_norm1"], [128, 8], stack=stack)
        t["n2"] = small(f"n2_{L}", W[f"l{L}_norm2"], [128, 8], stack=stack)
        t["gs1"] = K.sb(f"gs1_{L}", [128, 8], F32, stack)
        t["gs2"] = K.sb(f"gs2_{L}", [128, 8], F32, stack)
        return t

    def adaln_tasks(L, t):
        wm = W[f"l{L}_w_mod"]
        st = {}
        mod, bmod = t["mod"], t["bmod"]

        moda, modb = t["moda"], t["mod"]

        def step(g):
            if g == 0 or g == 4:
                st["bank"] = K.reserve()
            bank = st["bank"]
            if "next" in st:
                s_, v = st.pop("next")
            else:
                s_, v = ring.load(wview(wm, 0, 8, g * 512, 512), 8, 512, hold=True)
            if g + 1 < 12 and g != 3:
                st["next"] = ring.load(wview(wm, 0, 8, (g + 1) * 512, 512), 8, 512, hold=True)
            for oc in range(4):
                col = g * 4 + oc
                for kc in range(8):
                    K.mm(bank, bank.t[:, col:col + 1], v[:, kc, oc * 128:(oc + 1) * 128], siluc.t[:, kc:kc + 1],
                         r=(s_, siluc), start=(kc == 0), stop=(kc == 7),
                         first=(col in (0, 16) and kc == 0), last=(col in (15, 47) and kc == 7))
            s_.held = False
            if g == 3:
                K.op(dve, lambda h: h.tensor_tensor(out=moda.t[:], in0=bank.t[:, 0:16], in1=bmod.t[:, 0:16], op=ALU.add),
                     r=(bank, bmod), w=(moda,))
                K.release(bank)
                gs, n = t["gs1"], t["n1"]
                K.op(dve, lambda h: h.tensor_tensor(out=gs.t[:], in0=moda.t[:, 8:16], in1=n.t[:], op=ALU.mult), r=(moda, n), w=(gs,))
                K.op(dve, lambda h: h.tensor_tensor(out=gs.t[:], in0=gs.t[:], in1=n.t[:], op=ALU.add), r=(gs, n), w=(gs,))
            if g == 11:
                K.op(dve, lambda h: h.tensor_tensor(out=modb.t[:, 16:48], in0=bank.t[:, 16:48], in1=bmod.t[:, 16:48], op=ALU.add),
                     r=(bank, bmod), w=(modb,))
                K.release(bank)
                gs, n = t["gs2"], t["n2"]
                K.op(dve, lambda h: h.tensor_tensor(out=gs.t[:], in0=modb.t[:, 32:40], in1=n.t[:], op=ALU.mult), r=(modb, n), w=(gs,))
                K.op(dve, lambda h: h.tensor_tensor(out=gs.t[:], in0=gs.t[:], in1=n.t[:], op=ALU.add), r=(gs, n), w=(gs,))
        return [(lambda g=g: step(g)) for g in range(12)]

    def norm_mod(gs_ap, sh_ap, rtiles, stack_unused=None, nfeat=D, out_fn=None, router=None):
        stack = contextlib.ExitStack()

        def half(hh):
            bank = K.bank()
            if router is not None:
                rb = K.bank()
                router["banks"].append(rb)
            for c in range(8):
                sq = rotbuf("sq", 3, [128, 512], BF16, stack)
                K.op(act, lambda h, c=c, sq=sq: h.activation(out=sq.t[:], in_=X(c, hh), func=AF.Square),
                     r=(xh[c][hh],), w=(sq,))
                yield
                K.mm(bank, bank.t[:], ones_bf.t[:], sq.t[:], r=(sq, ones_bf), start=(c == 0), stop=(c == 7), finc=True)
                yield
            rstd = rotbuf("rstd", 2, [128, 512], F32, stack)
            lnv = rotbuf("lnv", 2, [128, 512], F32, stack)
            K.op(act, lambda h: h.activation(out=lnv.t[:], in_=bank.t[:], func=AF.Ln, bias=EPS, scale=1.0 / nfeat),
                 r=(bank,), w=(lnv,))
            yield
            K.op(act, lambda h: h.activation(out=rstd.t[:], in_=lnv.t[:], func=AF.Exp, scale=-0.5),
                 r=(lnv,), w=(rstd,))
            yield
            for c in range(8):
                tmp = rotbuf("nt", 3, [128, 512], F32, stack)
                K.op(dve, lambda h, c=c, tmp=tmp: h.tensor_tensor(out=tmp.t[:], in0=X(c, hh), in1=rstd.t[:], op=ALU.mult),
                     r=(xh[c][hh], rstd), w=(tmp,))
                yield
                if out_fn is None:
                    o_ap, o_t = H(c, hh), hh_[c][hh]
                else:
                    o_ap, o_t = out_fn(c, hh)
                sh = sh_ap(c) if sh_ap is not None else 0.0
                K.op(act, lambda h, c=c, tmp=tmp, o_ap=o_ap, sh=sh: h.activation(
                    out=o_ap, in_=tmp.t[:], func=AF.Identity, bias=sh, scale=gs_ap(c)),
                     r=(tmp,) + tuple(rtiles), w=(o_t,))
                yield
                if router is not None:
                    h2f = rotbuf("h2f", 2, [128, 512], F32, stack)
                    K.op(act, lambda h, c=c, tmp=tmp, h2f=h2f, sh=sh: h.activation(
                        out=h2f.t[:], in_=tmp.t[:], func=AF.Identity, bias=sh, scale=gs_ap(c)),
                         r=(tmp,) + tuple(rtiles), w=(h2f,))
                    yield
                    wr = router["wr"]
                    K.mm(rb, rb.t[0:8, :], wr.t[:, c, :], h2f.t[:], r=(wr, h2f), start=(c == 0), stop=(c == 7), finc=True)
                    yield
        zipper(half(0), half(1))
        K.barrier()
        stack.close()

    def proj(v, s, ncol0, M=128, rhs=None, rt=None, kcs=8):
        banks = []
        for hh in range(2):
            b = K.bank()
            for kc in range(kcs):
                if rhs is None:
                    r_ap, r_t = H(kc, hh), hh_[kc][hh]
                else:
                    r_ap, r_t = rhs(kc, hh), rt(kc, hh)
                K.mm(b, b.t[0:M, :], v[:, kc, ncol0:ncol0 + M], r_ap, r=(s, r_t), start=(kc == 0), stop=(kc == kcs - 1))
            banks.append(b)
        return banks

    HS = [slice(0, 512), slice(512, 1024)]

    def dbg_dump(i, ap, tiles):
        if debug:
            dt_ = dbg_tile
            K.op(dve, lambda h: h.tensor_copy(out=dt_.t[:], in_=ap), r=tuple(tiles), w=(dt_,))
            K.dma(sp, dbg_d[i], dt_.t[:], dt_, r=(dt_,))

    ad1 = adaln_alloc(1, None)
    rS = K.sb("rS", [128, 512], F32)
    L0 = contextlib.ExitStack()
    ad0 = adaln_alloc(0, L0)
    tsk0 = adaln_tasks(0, ad0)
    for tsk in tsk0[:4]:
        tsk()
    bg_tasks.extend(tsk0[4:])
    mod0, moda0, gs1, gs2 = ad0["mod"], ad0["moda"], ad0["gs1"], ad0["gs2"]
    norm_mod(lambda c: gs1.t[:, c:c + 1], lambda c: moda0.t[:, c:c + 1], (gs1, moda0), L0)

    MX = contextlib.ExitStack()
    conva = small("conva", W["l0_conv_a"], [128, 12], stack=MX)
    nconva = K.sb("nconva", [128, 12], F32, MX)
    K.op(dve, lambda h: h.tensor_scalar(out=nconva.t[:], in0=conva.t[:], scalar1=flags.t[:, 1:2], scalar2=None, op0=ALU.mult),
         r=(conva, flags), w=(nconva,))
    lcw = small("lcw", W["l0_lru_conv_w"], [128, 32], stack=MX)
    nlcw = K.sb("nlcw", [128, 32], F32, MX)
    K.op(dve, lambda h: h.tensor_scalar(out=nlcw.t[:], in0=lcw.t[:], scalar1=flags.t[:, 1:2], scalar2=None, op0=ALU.mult),
         r=(lcw, flags), w=(nlcw,))
    lcb = small("lcb", W["l0_lru_conv_b"], [128, 8], stack=MX)
    ba = small("ba", W["l0_lru_ba"], [128, 16], stack=MX)
    bi = small("bi", W["l0_lru_bi"], [128, 16], stack=MX)
    lam = small("lam", W["l0_lru_lambda"], [128, 16], stack=MX)
    h0 = small("h0", h0_d, [128, 16], stack=MX)
    wa = K.sb("wa", [128, 16, 128], BF16, MX)
    wi = K.sb("wi", [128, 16, 128], BF16, MX)
    K.dma(pool, wa.t[:], W["l0_lru_wa"].rearrange("(b h) k -> h b k", h=128), wa, w=(wa,))
    K.dma(pool, wi.t[:], W["l0_lru_wi"].rearrange("(b h) k -> h b k", h=128), wi, w=(wi,))
    lru_out = K.sb("lru_out", [128, 64], F32, MX)
    K.op(pool, lambda h: h.memset(lru_out.t[:], 0.0), w=(lru_out,))
    hba = K.sb("hba", [128, 16], F32, MX)
    hbi = K.sb("hbi", [128, 16], F32, MX)
    K.op(dve, lambda h: h.tensor_scalar(out=hba.t[:], in0=ba.t[:], scalar1=0.5, scalar2=None, op0=ALU.mult), r=(ba,), w=(hba,))
    K.op(dve, lambda h: h.tensor_scalar(out=hbi.t[:], in0=bi.t[:], scalar1=0.5, scalar2=None, op0=ALU.mult), r=(bi,), w=(hbi,))
    spv = K.sb("spv", [128, 16], F32, MX)
    m4 = K.sb("m4", [128, 16], F32, MX)
    m8 = K.sb("m8", [128, 16], F32, MX)
    K.op(act, lambda h: h.activation(out=spv.t[:], in_=lam.t[:], func=AF.Exp, scale=-1.0), r=(lam,), w=(spv,))
    K.op(act, lambda h: h.activation(out=spv.t[:], in_=spv.t[:], func=AF.Ln, bias=1.0, scale=1.0), r=(spv,), w=(spv,))
    K.op(dve, lambda h: h.tensor_scalar(out=m4.t[:], in0=spv.t[:], scalar1=-4.0, scalar2=None, op0=ALU.mult), r=(spv,), w=(m4,))
    K.op(dve, lambda h: h.tensor_scalar(out=m8.t[:], in0=spv.t[:], scalar1=-8.0, scalar2=None, op0=ALU.mult), r=(spv,), w=(m8,))

    w_in0 = W["l0_w_in"]
    pA = [ring.load(wview(w_in0, 0, 8, p * 512, 512), 8, 512, hold=True) for p in range(3)]
    MXA = contextlib.ExitStack()
    for j in range(4 if stage >= 2 else 0):
        bb = proj(pA[0][1], pA[0][0], j * 128)
        cc = proj(pA[1][1], pA[1][0], j * 128)
        xx = proj(pA[2][1], pA[2][0], j * 128)
        U = rotbuf("U3", 2, [128, T + 2], F32, MXA)
        if not getattr(U, "zeroed", False):
            K.op(pool, lambda h, U=U: h.memset(U.t[:], 0.0), w=(U,))
            U.zeroed = True
        tx = rotbuf("tx", 2, [128, T], F32, MXA)
        for hh in range(2):
            K.op(act, lambda h, hh=hh, tx=tx: h.activation(out=tx.t[:, HS[hh]], in_=xx[hh].t[:], func=AF.Copy),
                 r=(xx[hh],), w=(tx,))
            K.op(dve, lambda h, hh=hh, tx=tx, U=U: h.tensor_tensor(out=U.t[:, 1 + hh * 512:1 + (hh + 1) * 512], in0=cc[hh].t[:],
                                                                  in1=tx.t[:, HS[hh]], op=ALU.mult),
                 r=(cc[hh], tx), w=(U,))
        cv = rotbuf("cv", 2, [128, T], F32, MXA)
        K.op(act, lambda h, U=U, cv=cv: h.activation(out=cv.t[:], in_=U.t[:, 0:T], func=AF.Copy, scale=conva.t[:, j:j + 1]),
             r=(U, conva), w=(cv,))
        for k in (1, 2):
            K.op(dve, lambda h, k=k, U=U, cv=cv: h.scalar_tensor_tensor(out=cv.t[:], in0=U.t[:, k:k + T], scalar=conva.t[:, 4 * k + j:4 * k + j + 1],
                                                                      op0=ALU.mult, in1=cv.t[:], op1=ALU.add),
                 r=(U, conva, cv), w=(cv,))
        K.op(dve, lambda h, U=U, cv=cv: h.scalar_tensor_tensor(out=cv.t[:, 256:1024:256], in0=U.t[:, 256:1024:256], scalar=nconva.t[:, j:j + 1],
                                                            op0=ALU.mult, in1=cv.t[:, 256:1024:256], op1=ALU.add),
             r=(U, nconva, cv), w=(cv,))
        K.op(dve, lambda h, U=U, cv=cv: h.scalar_tensor_tensor(out=cv.t[:, 255:1023:256], in0=U.t[:, 257:1025:256], scalar=nconva.t[:, 8 + j:8 + j + 1],
                                                            op0=ALU.mult, in1=cv.t[:, 255:1023:256], op1=ALU.add),
             r=(U, nconva, cv), w=(cv,))
        for hh in range(2):
            K.op(dve, lambda h, hh=hh, cv=cv: h.tensor_tensor(out=ym[j].t[:, HS[hh]], in0=bb[hh].t[:], in1=cv.t[:, HS[hh]], op=ALU.mult),
                 r=(bb[hh], cv), w=(ym[j],))
        bg_step(2)

    for p_ in pA:
        p_[0].held = False
    K.barrier()
    MXA.close()
    pst = {}

    def halves(X):
        if not hasattr(X, "hv"):
            X.hv = [Tl(X.t, X.name + "_h0"), Tl(X.t, X.name + "_h1")]
        return X.hv

    def lru_F(j, out):
        if j % 4 == 0:
            for k_ in ("pG", "pX"):
                if k_ in pst:
                    pst[k_][0].held = False
            pst["pG"] = ring.load(wview(w_in0, 0, 8, 1536 + (j // 4) * 512, 512), 8, 512, hold=True)
            pst["pX"] = ring.load(wview(w_in0, 0, 8, 2560 + (j // 4) * 512, 512), 8, 512, hold=True)
        pG, pX = pst["pG"], pst["pX"]
        gg = proj(pG[1], pG[0], (j % 4) * 128)
        xb = proj(pX[1], pX[0], (j % 4) * 128)
        U = rotbuf("U4", 1, [128, T + 3], F32, MX)
        if not getattr(U, "zeroed", False):
            K.op(pool, lambda h, U=U: h.memset(U.t[:], 0.0), w=(U,))
            yield
            U.zeroed = True
        for hh in range(2):
            K.op(act, lambda h, hh=hh, U=U: h.activation(out=U.t[:, 2 + hh * 512:2 + (hh + 1) * 512], in_=xb[hh].t[:], func=AF.Copy),
                 r=(xb[hh],), w=(U,))
            yield
        gl = rotbuf("gl", 2, [128, T], F32, MX)
        for hh in range(2):
            sq = rotbuf("gsq", 1, [128, 512], F32, MX)
            K.op(act, lambda h, hh=hh, sq=sq: h.activation(out=sq.t[:], in_=gg[hh].t[:], func=AF.Square), r=(gg[hh],), w=(sq,))
            yield
            K.op(dve, lambda h, sq=sq: h.tensor_scalar(out=sq.t[:], in0=sq.t[:], scalar1=0.044715, scalar2=1.0, op0=ALU.mult, op1=ALU.add),
                 r=(sq,), w=(sq,))
            yield
            K.op(dve, lambda h, hh=hh, sq=sq: h.tensor_tensor(out=sq.t[:], in0=sq.t[:], in1=gg[hh].t[:], op=ALU.mult),
                 r=(sq, gg[hh]), w=(sq,))
            yield
            K.op(act, lambda h, sq=sq: h.activation(out=sq.t[:], in_=sq.t[:], func=AF.Tanh, scale=0.7978845608028654), r=(sq,), w=(sq,))
            yield
            K.op(dve, lambda h, hh=hh, sq=sq, gl=gl: h.scalar_tensor_tensor(out=gl.t[:, HS[hh]], in0=sq.t[:], scalar=1.0, op0=ALU.add,
                                                                          in1=gg[hh].t[:], op1=ALU.mult),
                 r=(sq, gg[hh]), w=(gl,))
            yield
        xc = rotbuf("xc", 2, [128, T], F32, MX)
        K.op(act, lambda h, U=U, xc=xc: h.activation(out=xc.t[:], in_=U.t[:, 0:T], func=AF.Identity, bias=lcb.t[:, j:j + 1],
                                                  scale=lcw.t[:, j:j + 1]),
             r=(U, lcw, lcb), w=(xc,))
        yield
        for k in (1, 2, 3):
            K.op(dve, lambda h, k=k, U=U, xc=xc: h.scalar_tensor_tensor(out=xc.t[:], in0=U.t[:, k:k + T], scalar=lcw.t[:, 8 * k + j:8 * k + j + 1],
                                                                      op0=ALU.mult, in1=xc.t[:], op1=ALU.add),
                 r=(U, lcw, xc), w=(xc,))
            yield
        for (ocol, ucol, k) in ((256, 256, 0), (256, 257, 1), (257, 257, 0), (255, 258, 3)):
            K.op(dve, lambda h, ocol=ocol, ucol=ucol, k=k, U=U, xc=xc: h.scalar_tensor_tensor(
                out=xc.t[:, ocol:ocol + 513:256], in0=U.t[:, ucol:ucol + 513:256], scalar=nlcw.t[:, 8 * k + j:8 * k + j + 1],
                op0=ALU.mult, in1=xc.t[:, ocol:ocol + 513:256], op1=ALU.add),
                 r=(U, nlcw, xc), w=(xc,))
            yield
        xcb = rotbuf("xcb", 1, [128, T], BF16, MX)
        K.op(act, lambda h, xc=xc, xcb=xcb: h.activation(out=xcb.t[:], in_=xc.t[:], func=AF.Copy), r=(xc,), w=(xcb,))
        yield
        out.update({"xcb": xcb, "xch": xc, "gl": gl})
        yield

    def lru_G(j, st):
        xcb = st["xcb"]
        thr_ = [rotbuf("thr", 2, [128, T], F32, MX) for _ in range(2)]
        thi_ = [rotbuf("thi", 2, [128, T], F32, MX) for _ in range(2)]
        for d in range(2):
            dj = d * 8 + j
            for (wt, th, hbias) in ((wa, thr_[d], hba), (wi, thi_[d], hbi)):
                for hh in range(2):
                    b = K.bank()
                    K.mm(b, b.t[:], wt.t[:, dj, :], xcb.t[:, HS[hh]], r=(wt, xcb), start=True, stop=True)
                    K.op(act, lambda h, b=b, th=th, hh=hh, hbias=hbias, dj=dj: h.activation(out=th.t[:, HS[hh]], in_=b.t[:], func=AF.Tanh,
                                                                                  bias=hbias.t[:, dj:dj + 1], scale=0.5),
                         r=(b, hbias), w=(halves(th)[hh],))
        st["thr_"], st["thi_"] = thr_, thi_

    def lru_B(j, st):
        xch, gl, thr_, thi_ = st["xch"], st["gl"], st["thr_"], st["thi_"]
        av_ = [rotbuf("av", 2, [128, T], F32, MX) for _ in range(2)]
        a2_ = [rotbuf("a2", 2, [128, T], F32, MX) for _ in range(2)]
        hd_l = [rotbuf("hd", 2, [128, T], F32, MX) for _ in range(2)]
        for d in range(2):
            dj = d * 8 + j
            thr, av, a2 = thr_[d], av_[d], a2_[d]
            for hh in range(2):
                K.op(act, lambda h, thr=thr, av=av, dj=dj, hh=hh: h.activation(out=av.t[:, HS[hh]], in_=thr.t[:, HS[hh]], func=AF.Exp,
                                                                            bias=m4.t[:, dj:dj + 1], scale=m4.t[:, dj:dj + 1]),
                     r=(halves(thr)[hh], m4), w=(halves(av)[hh],))
                yield
                K.op(act, lambda h, thr=thr, a2=a2, dj=dj, hh=hh: h.activation(out=a2.t[:, HS[hh]], in_=thr.t[:, HS[hh]], func=AF.Exp,
                                                                            bias=m8.t[:, dj:dj + 1], scale=m8.t[:, dj:dj + 1]),
                     r=(halves(thr)[hh], m8), w=(halves(a2)[hh],))
                yield
        for d in range(2):
            a2 = a2_[d]
            for hh in range(2):
                K.op(dve, lambda h, a2=a2, hh=hh: h.tensor_scalar(out=a2.t[:, HS[hh]], in0=a2.t[:, HS[hh]], scalar1=-1.0, scalar2=1.0, op0=ALU.mult, op1=ALU.add),
                     r=(halves(a2)[hh],), w=(halves(a2)[hh],))
                yield
                K.op(dve, lambda h, a2=a2, hh=hh: h.tensor_scalar(out=a2.t[:, HS[hh]], in0=a2.t[:, HS[hh]], scalar1=1e-30, scalar2=None, op0=ALU.max),
                     r=(halves(a2)[hh],), w=(halves(a2)[hh],))
                yield
        for d in range(2):
            a2 = a2_[d]
            for hh in range(2):
                K.op(act, lambda h, a2=a2, hh=hh: h.activation(out=a2.t[:, HS[hh]], in_=a2.t[:, HS[hh]], func=AF.Ln), r=(halves(a2)[hh],), w=(halves(a2)[hh],))
                yield
                K.op(act, lambda h, a2=a2, hh=hh: h.activation(out=a2.t[:, HS[hh]], in_=a2.t[:, HS[hh]], func=AF.Exp, bias=float(math.log(0.5)), scale=0.5), r=(halves(a2)[hh],), w=(halves(a2)[hh],))
                yield
        for d in range(2):
            thi, av, a2 = thi_[d], av_[d], a2_[d]
            for hh in range(2):
                K.op(dve, lambda h, a2=a2, hh=hh: h.tensor_tensor(out=a2.t[:, HS[hh]], in0=a2.t[:, HS[hh]], in1=xch.t[:, HS[hh]], op=ALU.mult),
                     r=(halves(a2)[hh], xch), w=(halves(a2)[hh],))
                yield
                K.op(dve, lambda h, a2=a2, thi=thi, hh=hh: h.scalar_tensor_tensor(out=a2.t[:, HS[hh]], in0=thi.t[:, HS[hh]], scalar=1.0, op0=ALU.add,
                                                                               in1=a2.t[:, HS[hh]], op1=ALU.mult),
                     r=(halves(a2)[hh], halves(thi)[hh]), w=(halves(a2)[hh],))
                yield
            for (hh, sl) in (((0, slice(256, 257)), (1, slice(512, 769, 256))) if d == 0 else ((0, slice(255, 512, 256)), (1, slice(767, 768)))):
                K.op(dve, lambda h, av=av, sl=sl: h.tensor_scalar(out=av.t[:, sl], in0=av.t[:, sl], scalar1=flags.t[:, 2:3], scalar2=None, op0=ALU.mult),
                     r=(halves(av)[hh], flags), w=(halves(av)[hh],))
                yield
        hdir = []
        for d in range(2):
            dj = d * 8 + j
            av, a2, hd = av_[d], a2_[d], hd_l[d]
            if d == 0:
                K.op(dve, lambda h, av=av, a2=a2, hd=hd, dj=dj: h.tensor_tensor_scan(out=hd.t[:, 0:512], data0=av.t[:, 0:512], data1=a2.t[:, 0:512],
                                                                                 initial=h0.t[:, dj:dj + 1], op0=ALU.mult, op1=ALU.add),
                     r=(halves(av)[0], halves(a2)[0], h0), w=(halves(hd)[0],))
                yield
                K.op(dve, lambda h, av=av, a2=a2, hd=hd: h.tensor_tensor_scan(out=hd.t[:, 512:1024], data0=av.t[:, 512:1024], data1=a2.t[:, 512:1024],
                                                                         initial=hd.t[:, 511:512], op0=ALU.mult, op1=ALU.add),
                     r=(halves(av)[1], halves(a2)[1], halves(hd)[0]), w=(halves(hd)[1],))
                yield
                K.op(pool, lambda h, hd=hd: h.tensor_copy(out=lru_out.t[:, j * 4:(j + 1) * 4], in_=hd.t[:, 255:1024:256]), r=(halves(hd)[0], halves(hd)[1]), w=(lru_out,))
                yield
            else:
                K.op(dve, lambda h, av=av, a2=a2, hd=hd, dj=dj: h.tensor_tensor_scan(out=hd.t[:, 1023:511:-1], data0=av.t[:, 1023:511:-1], data1=a2.t[:, 1023:511:-1],
                                                                                 initial=h0.t[:, dj:dj + 1], op0=ALU.mult, op1=ALU.add),
                     r=(halves(av)[1], halves(a2)[1], h0), w=(halves(hd)[1],))
                yield
                K.op(dve, lambda h, av=av, a2=a2, hd=hd: h.tensor_tensor_scan(out=hd.t[:, 511::-1], data0=av.t[:, 511::-1], data1=a2.t[:, 511::-1],
                                                                         initial=hd.t[:, 512:513], op0=ALU.mult, op1=ALU.add),
                     r=(halves(av)[0], halves(a2)[0], halves(hd)[1]), w=(halves(hd)[0],))
                yield
                K.op(pool, lambda h, hd=hd: h.tensor_copy(out=lru_out.t[:, 32 + j * 4:32 + (j + 1) * 4], in_=hd.t[:, 0:1024:256]), r=(halves(hd)[0], halves(hd)[1]), w=(lru_out,))
                yield
            hdir.append(hd)
        hf, hb = hdir
        for hh in range(2):
            K.op(dve, lambda h, hh=hh: h.tensor_tensor(out=hf.t[:, HS[hh]], in0=hf.t[:, HS[hh]], in1=hb.t[:, HS[hh]], op=ALU.add),
                 r=(halves(hf)[hh], halves(hb)[hh]), w=(halves(hf)[hh],))
            yield
            K.op(dve, lambda h, gl=gl, hh=hh: h.scalar_tensor_tensor(out=ym[4 + j].t[:, HS[hh]], in0=hf.t[:, HS[hh]], scalar=0.5, op0=ALU.mult, in1=gl.t[:, HS[hh]], op1=ALU.mult),
                 r=(halves(hf)[hh], gl), w=(ym[4 + j],))
            yield
    NJ = 8 if stage >= 3 else 0

    if NJ:
        stj = {}
        zipper(lru_F(0, stj), None)
        lru_G(0, stj)
        for j in range(NJ):
            nst = {} if j + 1 < NJ else None
            zipper(lru_F(j + 1, nst) if nst is not None else None, lru_B(j, stj))
            if nst is not None:
                lru_G(j + 1, nst)
            stj = nst
        for k_ in ("pG", "pX"):
            pst[k_][0].held = False
    K.dma(sp, lru_d, lru_out.t[:], lru_out, r=(lru_out,))
    K.barrier()
    MX.close()

    def out_proj(w_out, gate_ap, gate_t):
        for g in range(4):
            s, v = ring.load(wview(w_out, 0, 12, g * 256, 256), 12, 256)
            for o2 in range(2):
                oc = g * 2 + o2
                for hh in range(2):
                    b = K.bank()
                    for kc in range(12):
                        K.mm(b, b.t[:], v[:, kc, o2 * 128:(o2 + 1) * 128], ym[kc].t[:, HS[hh]], r=(s, ym[kc]), start=(kc == 0), stop=(kc == 11))
                    K.op(dve, lambda h, b=b, oc=oc, hh=hh: h.scalar_tensor_tensor(out=X(oc, hh), in0=b.t[:], scalar=gate_ap(oc), op0=ALU.mult,
                                                                               in1=X(oc, hh), op1=ALU.add),
                         r=(b, gate_t, xh[oc][hh]), w=(xh[oc][hh],))

    bg_step(1000)
    if stage >= 4:
        out_proj(W["l0_w_out"], lambda oc: mod0.t[:, 16 + oc:17 + oc], mod0)
    if debug:
        for c in range(2):
            dbg_dump(c, xs.t[:, c, :], (xh[c][0], xh[c][1]))

    def HFB(part, pc):
        return ym[part * 4 + pc // 2].t[:, (pc % 2) * 512:(pc % 2 + 1) * 512], ym[part * 4 + pc // 2]

    def hy_filter_tasks(stack):
        zin = small("zin", zin_d, [33, T], stack=stack)
        fw1 = small("fw1", W["l1_hy_f_w1"], [33, 64], stack=stack)
        fw2 = small("fw2", W["l1_hy_f_w2"], [64, 64], stack=stack)
        fw3 = small("fw3", W["l1_hy_f_w3"], [64, 1024], stack=stack)
        fb1 = small("fb1", W["l1_hy_f_b1"], [64, 1], stack=stack)
        fb2 = small("fb2", W["l1_hy_f_b2"], [64, 1], stack=stack)
        hid = [K.sb(f"hid{i}", [64, T], F32, stack) for i in range(2)]
        ta = K.sb("sin_a", [64, T], F32, stack)
        tb = K.sb("sin_b", [64, T], F32, stack)
        ti = K.sb("sin_i", [64, T], I32, stack)
        dv = decay_d.rearrange("(c p) n -> p c n", p=128)
        st = {"nmm": 0}
        tasks = []
        for li, (wt, bt, src, kdim) in enumerate(((fw1, fb1, zin, 33), (fw2, fb2, hid[0], 64))):
            def mlp_a(wt=wt, bt=bt, src=src, kdim=kdim):
                for hh in range(2):
                    bk = K.bank()
                    K.mm(bk, bk.t[0:64, :], wt.t[0:kdim, :], src.t[0:kdim, HS[hh]], r=(wt, src), start=True, stop=True)
                    K.op(act, lambda h, hh=hh, bk=bk: h.activation(out=ta.t[:, HS[hh]], in_=bk.t[0:64, :], func=AF.Identity, bias=bt.t[:, 0:1], scale=1.0),
                         r=(bk, bt), w=(ta,))

            def mlp_b(li=li):
                K.op(dve, lambda h: h.tensor_scalar(out=ta.t[:], in0=ta.t[:], scalar1=float(1.0 / (2 * math.pi)), scalar2=64.5, op0=ALU.mult, op1=ALU.add), r=(ta,), w=(ta,))
                K.op(dve, lambda h: h.tensor_copy(out=ti.t[:], in_=ta.t[:]), r=(ta,), w=(ti,))
                K.op(dve, lambda h: h.tensor_copy(out=tb.t[:], in_=ti.t[:]), r=(ti,), w=(tb,))
                K.op(dve, lambda h: h.tensor_tensor(out=ta.t[:], in0=ta.t[:], in1=tb.t[:], op=ALU.subtract), r=(ta, tb), w=(ta,))
                K.op(dve, lambda h: h.tensor_scalar(out=tb.t[:], in0=ta.t[:], scalar1=0.0, scalar2=None, op0=ALU.is_lt), r=(ta,), w=(tb,))
                K.op(dve, lambda h: h.tensor_tensor(out=ta.t[:], in0=ta.t[:], in1=tb.t[:], op=ALU.add), r=(ta, tb), w=(ta,))
                K.op(act, lambda h: h.activation(out=hid[li].t[:], in_=ta.t[:], func=AF.Sin, bias=float(-math.pi), scale=float(2 * math.pi)), r=(ta,), w=(hid[li],))
            tasks += [mlp_a, mlp_b]

        def hfil(pc):
            if pc == 0:
                st["sbank"] = K.reserve()
            sbank = st["sbank"]
            dec = rotbuf("dec", 2, [128, 512], F32, stack)
            K.dma(sp, dec.t[:], dv[:, pc, :], dec, w=(dec,))
            for part in range(2):
                bk = K.bank()
                K.mm(bk, bk.t[:], hid[1].t[:, pc * 128:(pc + 1) * 128], fw3.t[:, part * 512:(part + 1) * 512], r=(hid[1], fw3), start=True, stop=True)
                tmp = rotbuf("hft", 2, [128, 512], F32, stack)
                K.op(dve, lambda h, bk=bk, tmp=tmp, dec=dec: h.tensor_tensor(out=tmp.t[:], in0=bk.t[:], in1=dec.t[:], op=ALU.mult), r=(bk, dec), w=(tmp,))
                if part == 1 and pc == 0:
                    K.op(dve, lambda h, tmp=tmp: h.memset(tmp.t[0:1, :], 0.0), r=(tmp,), w=(tmp,))
                o_ap, o_t = HFB(part, pc)
                K.op(act, lambda h, tmp=tmp, o_ap=o_ap: h.activation(out=o_ap, in_=tmp.t[:], func=AF.Copy), r=(tmp,), w=(o_t,))
                ab = rotbuf("hfab", 2, [128, 512], BF16, stack)
                K.op(act, lambda h, tmp=tmp, ab=ab: h.activation(out=ab.t[:], in_=tmp.t[:], func=AF.Abs), r=(tmp,), w=(ab,))
                K.mm(sbank, sbank.t[:], ones_bf.t[:], ab.t[:], r=(ab, ones_bf), start=(st["nmm"] == 0), stop=(st["nmm"] == 15), finc=True)
                st["nmm"] += 1
        tasks += [(lambda pc=pc: hfil(pc)) for pc in range(8)]

        def fin():
            sbank = st["sbank"]
            K.op(dve, lambda h: h.reciprocal(out=rS.t[:], in_=sbank.t[:]), r=(sbank,), w=(rS,))
            K.release(sbank)
        tasks.append(fin)
        return tasks

    def ffn_unit(Wg, Wu, Wd, gcol0, drow0, gate_ap, gate_t, stack, bg=None):
        actt = [rotbuf("actt", 11, [128, T], BF16, stack) for _ in range(11)]
        jj = 0
        for (c0, n) in ((0, 512), (512, 512), (1024, 384)):
            sg_, vg = ring.load(wview(Wg, 0, 8, gcol0 + c0, n), 8, n, hold=True)
            su_, vu = ring.load(wview(Wu, 0, 8, gcol0 + c0, n), 8, n, hold=True)
            for jc in range(n // 128):
                bgk = proj(vg, sg_, jc * 128)
                buk = proj(vu, su_, jc * 128)
                for hh in range(2):
                    sgt = rotbuf("sgt", 3, [128, 512], F32, stack)
                    K.op(act, lambda h, hh=hh, sgt=sgt: h.activation(out=sgt.t[:], in_=bgk[hh].t[:], func=AF.Silu), r=(bgk[hh],), w=(sgt,))
                    if bg is None:
                        K.op(dve, lambda h, hh=hh, sgt=sgt, jj=jj: h.tensor_tensor(out=actt[jj].t[:, HS[hh]], in0=sgt.t[:], in1=buk[hh].t[:], op=ALU.mult),
                             r=(sgt, buk[hh]), w=(actt[jj],))
                    else:
                        K.op(dve, lambda h, hh=hh, sgt=sgt: h.tensor_tensor(out=sgt.t[:], in0=sgt.t[:], in1=buk[hh].t[:], op=ALU.mult),
                             r=(sgt, buk[hh]), w=(sgt,))
                        K.op(dve, lambda h, hh=hh, sgt=sgt, jj=jj: h.tensor_tensor(out=actt[jj].t[:, HS[hh]], in0=sgt.t[:], in1=bg.t[:, HS[hh]], op=ALU.mult),
                             r=(sgt, bg), w=(actt[jj],))
                jj += 1
                if jc == n // 128 - 1:
                    sg_.held = False
                    su_.held = False
                bg_step()
        for g in range(4):
            s, v = ring.load(wview(Wd, drow0, 11, g * 256, 256), 11, 256)
            for o2 in range(2):
                oc = g * 2 + o2
                for hh in range(2):
                    b = K.bank()
                    for kc in range(11):
                        K.mm(b, b.t[:], v[:, kc, o2 * 128:(o2 + 1) * 128], actt[kc].t[:, HS[hh]], r=(s, actt[kc]), start=(kc == 0), stop=(kc == 10))
                    K.op(dve, lambda h, b=b, oc=oc, hh=hh: h.scalar_tensor_tensor(out=X(oc, hh), in0=b.t[:], scalar=gate_ap(oc), op0=ALU.mult,
                                                                               in1=X(oc, hh), op1=ALU.add),
                         r=(b, gate_t, xh[oc][hh]), w=(xh[oc][hh],))

    norm_mod(lambda c: gs2.t[:, c:c + 1], lambda c: mod0.t[:, 24 + c:25 + c], (gs2, mod0), L0)
    FF = contextlib.ExitStack()
    if stage >= 6:
        bg_tasks.extend(hy_filter_tasks(FF))
        bg_tasks.extend(adaln_tasks(1, ad1))
    for u in range(2 if stage >= 5 else 0):
        ffn_unit(W["l0_ffn_gate"], W["l0_ffn_up"], W["l0_ffn_down"], 1408 * u, 1408 * u,
                 lambda oc: mod0.t[:, 40 + oc:41 + oc], mod0, FF)
    bg_step(1000)
    K.barrier()
    FF.close()
    L0.close()
    if debug:
        for c in range(2):
            dbg_dump(2 + c, xs.t[:, c, :], (xh[c][0], xh[c][1]))


    if stage >= 6:
        L1 = contextlib.ExitStack()
        bg_step(1000)
        mod1, moda1, gs1b, gs2b = ad1["mod"], ad1["moda"], ad1["gs1"], ad1["gs2"]
        norm_mod(lambda c: gs1b.t[:, c:c + 1], lambda c: moda1.t[:, c:c + 1], (gs1b, moda1))
        SC = 1.0 / math.sqrt(192.0)
        cqn = [K.sb(f"cqn{c}", [128, T], BF16, L1) for c in range(3)]
        ckv_all = [K.sb(f"ckva{c}", [128, LK], BF16, L1) for c in range(2)]
        kr_all = K.sb("kr_all", [128, LK], BF16, L1)
        K.op(pool, lambda h: h.memset(kr_all.t[64:128, :], 0.0), w=(kr_all,))
        K.dma(sp, kr_all.t[64:68, :], mk_l_d, kr_all, r=(kr_all,), w=(kr_all,))
        w_in1 = W["l1_w_in"]

        def small_norm(src, n, nfeat, gain, emit, stack):
            def half(hh):
                    bank = K.bank()
                    for c in range(n):
                        sq = rotbuf("sq", 3, [128, 512], BF16, stack)
                        K.op(act, lambda h, c=c, sq=sq: h.activation(out=sq.t[:], in_=src[c].t[:, HS[hh]], func=AF.Square), r=(src[c],), w=(sq,))
                        yield
                        K.mm(bank, bank.t[:], ones_bf.t[:], sq.t[:], r=(sq, ones_bf), start=(c == 0), stop=(c == n - 1), finc=True)
                        yield
                    rstd = rotbuf("rstd", 2, [128, 512], F32, stack)
                    K.op(act, lambda h: h.activation(out=rstd.t[:], in_=bank.t[:], func=AF.Ln, bias=EPS, scale=1.0 / nfeat), r=(bank,), w=(rstd,))
                    yield
                    K.op(act, lambda h: h.activation(out=rstd.t[:], in_=rstd.t[:], func=AF.Exp, scale=-0.5), r=(rstd,), w=(rstd,))
                    yield
                    for c in range(n):
                        tmp = rotbuf("nt", 3, [128, 512], F32, stack)
                        K.op(dve, lambda h, c=c, tmp=tmp: h.tensor_tensor(out=tmp.t[:], in0=src[c].t[:, HS[hh]], in1=rstd.t[:], op=ALU.mult),
                             r=(src[c], rstd), w=(tmp,))
                        yield
                        emit(c, hh, tmp, gain)
                        yield
            zipper(half(0), half(1))

        P1 = contextlib.ExitStack()
        qn_g = small("qn_g", W["l1_q_norm"], [128, 3], stack=P1)
        kvn_g = small("kvn_g", W["l1_kv_norm"], [128, 2], stack=P1)
        rcos = small("rcos", ropecos_d, [64, T], stack=P1)
        rsin = small("rsin", ropesin_d, [64, T], stack=P1)
        cq = [K.sb(f"cq{c}", [128, T], F32, P1) for c in range(3)]
        ckr = [K.sb(f"ckr{c}", [128, T], F32, P1) for c in range(2)]
        ckvn = K.sb("ckvn", [128, 2, T], F32, P1)
        krf = K.sb("krf", [64, T], F32, P1)
        ctxf = K.sb("ctxf", [128, 2, LCTX], F32, P1)
        ctxk = K.sb("ctxk", [64, LCTX], F32, P1)
        K.dma(sp, ctxf.t[:], ctx_ckv_d.rearrange("(c p) t -> p c t", p=128), ctxf, w=(ctxf,))
        K.dma(sp, ctxk.t[:], ctx_kr_d, ctxk, w=(ctxk,))
        for c in range(2):
            K.op(act, lambda h, c=c: h.activation(out=ckv_all[c].t[:, 0:LCTX], in_=ctxf.t[:, c, :], func=AF.Copy), r=(ctxf,), w=(ckv_all[c],))
        K.op(act, lambda h: h.activation(out=kr_all.t[0:64, 0:LCTX], in_=ctxk.t[:], func=AF.Copy), r=(ctxk,), w=(kr_all,))
        pa = ring.load(wview(w_in1, 0, 8, 0, 512), 8, 512)
        pb = ring.load(wview(w_in1, 0, 8, 512, 192), 8, 192)
        pc_ = ring.load(wview(W["l1_w_in_krsw"], 0, 8, 0, 64), 8, 64)
        for c in range(3):
            bk = proj(pa[1], pa[0], c * 128)
            for hh in range(2):
                K.op(act, lambda h, c=c, hh=hh: h.activation(out=cq[c].t[:, HS[hh]], in_=bk[hh].t[:], func=AF.Copy), r=(bk[hh],), w=(cq[c],))
        for c in range(2):
            bk = proj(pa[1], pa[0], 384) if c == 0 else proj(pb[1], pb[0], 0)
            for hh in range(2):
                K.op(act, lambda h, c=c, hh=hh: h.activation(out=ckr[c].t[:, HS[hh]], in_=bk[hh].t[:], func=AF.Copy), r=(bk[hh],), w=(ckr[c],))
        bkr = proj(pb[1], pb[0], 128, M=64)
        bks = proj(pc_[1], pc_[0], 0, M=64)
        for hh in range(2):
            K.op(act, lambda h, hh=hh: h.activation(out=krf.t[:, HS[hh]], in_=bkr[hh].t[0:64, :], func=AF.Copy), r=(bkr[hh],), w=(krf,))
            t1 = rotbuf("rt1", 1, [64, 512], F32, P1)
            t2 = rotbuf("rt2", 1, [64, 512], F32, P1)
            K.op(dve, lambda h, hh=hh, t1=t1: h.tensor_tensor(out=t1.t[:], in0=bkr[hh].t[0:64, :], in1=rcos.t[:, HS[hh]], op=ALU.mult), r=(bkr[hh], rcos), w=(t1,))
            K.op(dve, lambda h, hh=hh, t2=t2: h.tensor_tensor(out=t2.t[:], in0=bks[hh].t[0:64, :], in1=rsin.t[:, HS[hh]], op=ALU.mult), r=(bks[hh], rsin), w=(t2,))
            K.op(dve, lambda h, hh=hh, t1=t1, t2=t2: h.tensor_tensor(out=kr_all.t[0:64, LCTX + hh * 512:LCTX + (hh + 1) * 512], in0=t1.t[:], in1=t2.t[:], op=ALU.add),
                 r=(t1, t2), w=(kr_all,))
        K.dma(sp, kr_out_d, krf.t[:], krf, r=(krf,))

        def emit_cq(c, hh, tmp, gain):
            K.op(act, lambda h: h.activation(out=cqn[c].t[:, HS[hh]], in_=tmp.t[:], func=AF.Copy, scale=gain.t[:, c:c + 1]), r=(tmp, gain), w=(cqn[c],))

        def emit_ckv(c, hh, tmp, gain):
            K.op(act, lambda h: h.activation(out=ckvn.t[:, c, HS[hh]], in_=tmp.t[:], func=AF.Copy, scale=gain.t[:, c:c + 1]), r=(tmp, gain), w=(ckvn,))
            K.op(act, lambda h: h.activation(out=ckv_all[c].t[:, LCTX + hh * 512:LCTX + (hh + 1) * 512], in_=tmp.t[:], func=AF.Copy, scale=gain.t[:, c:c + 1]),
                 r=(tmp, gain), w=(ckv_all[c],))
        small_norm(cq, 3, 384, qn_g, emit_cq, P1)
        small_norm(ckr, 2, 256, kvn_g, emit_ckv, P1)
        K.dma(sp, ckv_out_d.rearrange("(c p) t -> p c t", p=128), ckvn.t[:], ckvn, r=(ckvn,))
        K.barrier()
        P1.close()

        P2 = contextlib.ExitStack()
        hsw = small("hsw", W["l1_hy_short_w"], [128, 36], stack=P2)
        nhsw = K.sb("nhsw", [128, 36], F32, P2)
        K.op(dve, lambda h: h.tensor_scalar(out=nhsw.t[:], in0=hsw.t[:], scalar1=flags.t[:, 1:2], scalar2=None, op0=ALU.mult), r=(hsw, flags), w=(nhsw,))
        hsb = small("hsb", W["l1_hy_short_b"], [128, 12], stack=P2)
        zf = [K.sb(f"zf{j}", [128, T], F32, P2) for j in range(4)]
        pu = [ring.load(wview(w_in1, 0, 8, 704 + p * 512, 512), 8, 512) for p in range(3)]

        def uh_conv(c, out_ap, out_tile, mul_tile=None):
            bk = proj(pu[c // 4][1], pu[c // 4][0], (c % 4) * 128)
            U = rotbuf("Uh", 2, [128, T + 2], F32, P2)
            if not getattr(U, "zeroed", False):
                K.op(pool, lambda h, U=U: h.memset(U.t[:], 0.0), w=(U,))
                yield
                U.zeroed = True
            for hh in range(2):
                K.op(act, lambda h, hh=hh: h.activation(out=U.t[:, 1 + hh * 512:1 + (hh + 1) * 512], in_=bk[hh].t[:], func=AF.Copy), r=(bk[hh],), w=(U,))
                yield
            cv = rotbuf("cvh", 2, [128, T], F32, P2)
            K.op(act, lambda h: h.activation(out=cv.t[:], in_=U.t[:, 0:T], func=AF.Identity, bias=hsb.t[:, c:c + 1], scale=hsw.t[:, c:c + 1]),
                 r=(U, hsw, hsb), w=(cv,))
            yield
            for k in (1, 2):
                K.op(dve, lambda h, k=k: h.scalar_tensor_tensor(out=cv.t[:], in0=U.t[:, k:k + T], scalar=hsw.t[:, 12 * k + c:12 * k + c + 1],
                                                             op0=ALU.mult, in1=cv.t[:], op1=ALU.add), r=(U, hsw, cv), w=(cv,))
                yield
            K.op(dve, lambda h: h.scalar_tensor_tensor(out=cv.t[:, 256:1024:256], in0=U.t[:, 256:1024:256], scalar=nhsw.t[:, c:c + 1],
                                                    op0=ALU.mult, in1=cv.t[:, 256:1024:256], op1=ALU.add), r=(U, nhsw, cv), w=(cv,))
            yield
            K.op(dve, lambda h: h.scalar_tensor_tensor(out=cv.t[:, 255:1023:256], in0=U.t[:, 257:1025:256], scalar=nhsw.t[:, 24 + c:24 + c + 1],
                                                    op0=ALU.mult, in1=cv.t[:, 255:1023:256], op1=ALU.add), r=(U, nhsw, cv), w=(cv,))
            yield
            if mul_tile is None:
                K.op(act, lambda h: h.activation(out=out_ap, in_=cv.t[:], func=AF.Copy), r=(cv,), w=(out_tile,))
                yield
            else:
                K.op(dve, lambda h: h.tensor_tensor(out=out_ap, in0=cv.t[:], in1=mul_tile.t[:], op=ALU.mult), r=(cv, mul_tile), w=(out_tile,))
                yield

        def zchain(j):
            yield from uh_conv(4 + j, zf[j].t[:], zf[j])
            yield from uh_conv(8 + j, zf[j].t[:], zf[j], mul_tile=zf[j])
        for j in (0, 2):
            zipper(uh_conv(j, ym[8 + j].t[:], ym[8 + j]), uh_conv(j + 1, ym[9 + j].t[:], ym[9 + j]))
        for j in (0, 2):
            zipper(zchain(j), zchain(j + 1))
        for j in range(4):
            K.op(act, lambda h, j=j: h.activation(out=hs.t[:, j, :], in_=zf[j].t[:], func=AF.Copy), r=(zf[j],), w=(hh_[j][0], hh_[j][1]))
        for tc in range(8):
            bk = K.bank()
            for j in range(4):
                K.transpose(bk, bk.t[:, j * 128:(j + 1) * 128], zf[j].t[:, tc * 128:(tc + 1) * 128], ident.t[:], r=(zf[j], ident),
                            first=(j == 0), last=(j == 3))
            K.op(act, lambda h, tc=tc, bk=bk: h.activation(out=hs.t[:, 4 + tc // 2, (tc % 2) * 512:(tc % 2 + 1) * 512], in_=bk.t[:], func=AF.Copy),
                 r=(bk,), w=(hh_[4 + tc // 2][tc % 2],))
        K.barrier()
        P2.close()

        def ZTM(tc):
            return hs.t[:, 4 + tc // 2, (tc % 2) * 512:(tc % 2 + 1) * 512], hh_[4 + tc // 2][tc % 2]

        P3 = contextlib.ExitStack()
        Kt = [K.sb(f"Kq{q}", [128, 512], F32, P3) for q in range(4)]
        Ys = [[K.sb(f"Ys{j}_{q}", [128, 512], BF16, P3) for q in range(4)] for j in range(4)]
        yacc = [K.sb(f"yacc{j}", [128, T], F32, P3) for j in range(4)]
        hyb = small("hyb", W["l1_hy_bias"], [128, 4], stack=P3)
        Fv = Fm_d.rearrange("(c p) n -> p c n", p=128)
        for p in range(4):
            fs, fvw = ring.load(Fv[:, :, p * 512:(p + 1) * 512], 8, 512, q=sp)
            for q in range(4):
                bf_, bb_ = K.bank(), K.bank()
                for part, bk in ((0, bf_), (1, bb_)):
                    for pc in range(8):
                        h_ap, h_t = HFB(part, pc)
                        K.mm(bk, bk.t[:], fvw[:, pc, q * 128:(q + 1) * 128], h_ap, r=(fs, h_t), start=(pc == 0), stop=(pc == 7))
                hbs = rotbuf("hbs", 1, [128, 512], F32, P3)
                K.op(act, lambda h, hbs=hbs, bb_=bb_: h.activation(out=hbs.t[:], in_=bb_.t[:], func=AF.Copy), r=(bb_,), w=(hbs,))
                K.op(dve, lambda h, q=q, hbs=hbs, bf_=bf_: h.tensor_tensor(out=Kt[q].t[:], in0=bf_.t[:], in1=hbs.t[:], op=(ALU.add if q % 2 == 0 else ALU.subtract)),
                     r=(bf_, hbs), w=(Kt[q],))
                if p == 0 and q == 1:
                    K.op(dve, lambda h, hbs=hbs, bf_=bf_: h.tensor_tensor(out=Kt[1].t[0:1, :], in0=bf_.t[0:1, :], in1=hbs.t[0:1, :], op=ALU.add),
                         r=(bf_, hbs, Kt[1]), w=(Kt[1],))
                K.op(dve, lambda h, q=q: h.tensor_tensor(out=Kt[q].t[:], in0=Kt[q].t[:], in1=rS.t[:], op=ALU.mult), r=(Kt[q], rS), w=(Kt[q],))
            for j in range(4):
                zb = []
                for q in range(4):
                    bk = K.bank()
                    for i2, tc in enumerate((2 * j, 2 * j + 1)):
                        z_ap, z_t = ZTM(tc)
                        K.mm(bk, bk.t[:], fvw[:, tc, q * 128:(q + 1) * 128], z_ap, r=(fs, z_t), start=(i2 == 0), stop=(i2 == 1))
                    zs = rotbuf("zs", 7, [128, 512], F32, P3)
                    K.op(act, lambda h, zs=zs, bk=bk: h.activation(out=zs.t[:], in_=bk.t[:], func=AF.Copy), r=(bk,), w=(zs,))
                    zb.append(zs)
                for cc in range(2):
                    zre, zim, kre, kim = zb[2 * cc], zb[2 * cc + 1], Kt[2 * cc], Kt[2 * cc + 1]
                    t1 = rotbuf("sp1", 2, [128, 512], F32, P3)
                    t2 = rotbuf("sp2", 2, [128, 512], F32, P3)
                    K.op(dve, lambda h, t1=t1, kre=kre, zre=zre: h.tensor_tensor(out=t1.t[:], in0=kre.t[:], in1=zre.t[:], op=ALU.mult), r=(kre, zre), w=(t1,))
                    K.op(dve, lambda h, t2=t2, kim=kim, zim=zim: h.tensor_tensor(out=t2.t[:], in0=kim.t[:], in1=zim.t[:], op=ALU.mult), r=(kim, zim), w=(t2,))
                    yre, yim = Ys[j][2 * cc], Ys[j][2 * cc + 1]
                    K.op(dve, lambda h, t1=t1, t2=t2, yre=yre: h.tensor_tensor(out=yre.t[:], in0=t1.t[:], in1=t2.t[:], op=ALU.subtract), r=(t1, t2), w=(yre,))
                    if p == 0 and cc == 0:
                        K.op(dve, lambda h, t1=t1, yre=yre: h.tensor_copy(out=yre.t[0:1, :], in_=t1.t[0:1, :]), r=(t1, yre), w=(yre,))
                    t3 = rotbuf("sp1", 2, [128, 512], F32, P3)
                    t4 = rotbuf("sp2", 2, [128, 512], F32, P3)
                    K.op(dve, lambda h, t3=t3, kre=kre, zim=zim: h.tensor_tensor(out=t3.t[:], in0=kre.t[:], in1=zim.t[:], op=ALU.mult), r=(kre, zim), w=(t3,))
                    K.op(dve, lambda h, t4=t4, kim=kim, zre=zre: h.tensor_tensor(out=t4.t[:], in0=kim.t[:], in1=zre.t[:], op=ALU.mult), r=(kim, zre), w=(t4,))
                    K.op(dve, lambda h, t3=t3, t4=t4, yim=yim: h.tensor_tensor(out=yim.t[:], in0=t3.t[:], in1=t4.t[:], op=ALU.add), r=(t3, t4), w=(yim,))
                    if p == 0 and cc == 0:
                        K.op(dve, lambda h, t2=t2, yim=yim: h.tensor_copy(out=yim.t[0:1, :], in_=t2.t[0:1, :]), r=(t2, yim), w=(yim,))
            ifs = []
            for j in range(4):
                ifs.append(ring.load(iFm_d[j, p * 512:(p + 1) * 512, :].rearrange("(q f) t -> f q t", f=128), 4, T, q=sp))
            for jo in range(4):
                for hh in range(2):
                    bk = K.bank()
                    n = 0
                    for j in range(4):
                        for q in range(4):
                            K.mm(bk, bk.t[:], Ys[j][q].t[:, jo * 128:(jo + 1) * 128], ifs[j][1][:, q, hh * 512:(hh + 1) * 512], r=(Ys[j][q], ifs[j][0]),
                                 start=(n == 0), stop=(n == 15))
                            n += 1
                    if p == 0:
                        K.op(act, lambda h, jo=jo, hh=hh, bk=bk: h.activation(out=yacc[jo].t[:, HS[hh]], in_=bk.t[:], func=AF.Copy), r=(bk,), w=(yacc[jo],))
                    else:
                        K.op(dve, lambda h, jo=jo, hh=hh, bk=bk: h.tensor_tensor(out=yacc[jo].t[:, HS[hh]], in0=bk.t[:], in1=yacc[jo].t[:, HS[hh]], op=ALU.add),
                             r=(bk, yacc[jo]), w=(yacc[jo],))
        for jo in range(4):
            K.op(dve, lambda h, jo=jo: h.scalar_tensor_tensor(out=yacc[jo].t[:], in0=hs.t[:, jo, :], scalar=hyb.t[:, jo:jo + 1], op0=ALU.mult,
                                                           in1=yacc[jo].t[:], op1=ALU.add), r=(hh_[jo][0], hh_[jo][1], hyb, yacc[jo]), w=(yacc[jo],))
            K.op(dve, lambda h, jo=jo: h.tensor_tensor(out=ym[8 + jo].t[:], in0=ym[8 + jo].t[:], in1=yacc[jo].t[:], op=ALU.mult),
                 r=(ym[8 + jo], yacc[jo]), w=(ym[8 + jo],))
        K.barrier()
        P3.close()

        P4 = contextlib.ExitStack()
        rcos = small("rcos", ropecos_d, [64, T], stack=P4)
        rsin = small("rsin", ropesin_d, [64, T], stack=P4)
        qr_bufs = [K.sb(f"qr{i}", [128, T], BF16, P4) for i in range(2)]
        for qb in qr_bufs:
            K.op(pool, lambda h, qb=qb: h.memset(qb.t[:], 0.0), w=(qb,))
            K.dma(sp, qb.t[64:68, :], mk_r_d, qb, r=(qb,), w=(qb,))
        vt = [K.sb(f"v{kt}", [128, 1024], BF16, P4) for kt in range(NKT)]
        wqn = ring.load(wview(W["l1_wq_nope"], 0, 3, 0, 1024), 3, 1024)
        wqr = ring.load(wview(W["l1_wq_rope"], 0, 3, 0, 512), 3, 512)
        wqs = ring.load(wview(W["l1_wq_rope_sw"], 0, 3, 0, 512), 3, 512)
        wkn = ring.load(wview(W["l1_wk_nope"], 0, 2, 0, 1024), 2, 1024)
        wvv = ring.load(wview(W["l1_wv"], 0, 2, 0, 1024), 2, 1024)
        for kt in range(NKT):
            for c2 in range(2):
                bk = K.bank()
                for kc in range(2):
                    K.mm(bk, bk.t[:], ckv_all[kc].t[:, kt * 128:(kt + 1) * 128], wvv[1][:, kc, c2 * 512:(c2 + 1) * 512], r=(ckv_all[kc], wvv[0]),
                         start=(kc == 0), stop=(kc == 1))
                K.op(act, lambda h, kt=kt, c2=c2, bk=bk: h.activation(out=vt[kt].t[:, c2 * 512:(c2 + 1) * 512], in_=bk.t[:], func=AF.Copy), r=(bk,), w=(vt[kt],))
        def head_proj(hd_):
            qn = rotbuf("qn", 2, [128, T], BF16, P4)
            qr = qr_bufs[hd_ % 2]
            kn = rotbuf("kn", 2, [128, LK], BF16, P4)
            bq = proj(wqn[1], wqn[0], hd_ * 128, rhs=lambda kc, hh: cqn[kc].t[:, HS[hh]], rt=lambda kc, hh: cqn[kc], kcs=3)
            for hh in range(2):
                K.op(act, lambda h, hh=hh, qn=qn: h.activation(out=qn.t[:, HS[hh]], in_=bq[hh].t[:], func=AF.Copy), r=(bq[hh],), w=(qn,))
            br = proj(wqr[1], wqr[0], hd_ * 64, M=64, rhs=lambda kc, hh: cqn[kc].t[:, HS[hh]], rt=lambda kc, hh: cqn[kc], kcs=3)
            bs = proj(wqs[1], wqs[0], hd_ * 64, M=64, rhs=lambda kc, hh: cqn[kc].t[:, HS[hh]], rt=lambda kc, hh: cqn[kc], kcs=3)
            for hh in range(2):
                t1 = rotbuf("rt1", 2, [64, 512], F32, P4)
                t2 = rotbuf("rt2", 2, [64, 512], F32, P4)
                K.op(dve, lambda h, hh=hh, t1=t1: h.tensor_tensor(out=t1.t[:], in0=br[hh].t[0:64, :], in1=rcos.t[:, HS[hh]], op=ALU.mult), r=(br[hh], rcos), w=(t1,))
                K.op(dve, lambda h, hh=hh, t2=t2: h.tensor_tensor(out=t2.t[:], in0=bs[hh].t[0:64, :], in1=rsin.t[:, HS[hh]], op=ALU.mult), r=(bs[hh], rsin), w=(t2,))
                K.op(dve, lambda h, hh=hh, t1=t1, t2=t2, qr=qr: h.tensor_tensor(out=qr.t[0:64, HS[hh]], in0=t1.t[:], in1=t2.t[:], op=ALU.add), r=(t1, t2), w=(qr,))
            for g3 in range(3):
                bk = K.bank()
                for kc in range(2):
                    K.mm(bk, bk.t[:], wkn[1][:, kc, hd_ * 128:(hd_ + 1) * 128], ckv_all[kc].t[:, g3 * 512:(g3 + 1) * 512], r=(wkn[0], ckv_all[kc]),
                         start=(kc == 0), stop=(kc == 1))
                K.op(act, lambda h, g3=g3, bk=bk, kn=kn: h.activation(out=kn.t[:, g3 * 512:(g3 + 1) * 512], in_=bk.t[:], func=AF.Copy), r=(bk,), w=(kn,))
            return qn, qr, kn

        nxt = head_proj(0)
        for hd_ in range(8):
            qn, qr, kn = nxt
            for hh in range(2):
                ob = K.banks[0 + 2 * (hh % 2)]
                db = K.banks[1 + 2 * (hh % 2)]

                def s_mm(kt, hh=hh):
                    sbk = K.banks[4 + (kt % 4)]
                    K.mm(sbk, sbk.t[:], kn.t[:, kt * 128:(kt + 1) * 128], qn.t[:, HS[hh]], r=(kn, qn), start=True, stop=False)
                    K.mm(sbk, sbk.t[:], kr_all.t[:, kt * 128:(kt + 1) * 128], qr.t[:, HS[hh]], r=(kr_all, qr), start=False, stop=True)
                s_mm(0)
                s_mm(1)
                s_mm(2)
                for kt in range(NKT):
                    sbk = K.banks[4 + (kt % 4)]
                    pT = rotbuf("pT", 4, [128, 512], BF16, P4)
                    K.op(act, lambda h, sbk=sbk, pT=pT: h.activation(out=pT.t[:], in_=sbk.t[:], func=AF.Exp, scale=SC), r=(sbk,), w=(pT,))
                    if kt + 3 < NKT:
                        s_mm(kt + 3)
                    K.mm(ob, ob.t[:], vt[kt].t[:, hd_ * 128:(hd_ + 1) * 128], pT.t[:], r=(vt[kt], pT), start=(kt == 0), stop=(kt == NKT - 1), finc=True)
                    K.mm(db, db.t[:], ones_bf.t[:], pT.t[:], r=(ones_bf, pT), start=(kt == 0), stop=(kt == NKT - 1), finc=True)
                rec = rotbuf("rec", 2, [128, 512], F32, P4)
                K.op(dve, lambda h, rec=rec, db=db: h.reciprocal(out=rec.t[:], in_=db.t[:]), r=(db,), w=(rec,))
                K.op(dve, lambda h, hh=hh, rec=rec, ob=ob, hd_=hd_: h.tensor_tensor(out=ym[hd_].t[:, HS[hh]], in0=ob.t[:], in1=rec.t[:], op=ALU.mult),
                     r=(ob, rec), w=(ym[hd_],))
                if hh == 0 and hd_ + 1 < 8:
                    nxt = head_proj(hd_ + 1)
        K.barrier()
        P4.close()
        out_proj(W["l1_w_out"], lambda oc: mod1.t[:, 16 + oc:17 + oc], mod1)
        if debug:
            for c in range(2):
                dbg_dump(4 + c, xs.t[:, c, :], (xh[c][0], xh[c][1]))

        MO = contextlib.ExitStack()
        wr = K.sb("wr", [128, 8, 8], F32, MO)
        K.dma(sp, wr.t[:], W["l1_router_w"].rearrange("(c p) e -> p c e", p=128), wr, w=(wr,))
        rbias = small("rbias", W["l1_router_b"], [8, 1], stack=MO)
        sel8 = small("sel8", sel8_d, [8, 1024], dt=BF16, stack=MO)
        lg = K.sb("lg", [8, T], F32, MO)
        lt = K.sb("lt", [128, 64], F32, MO)
        gts = K.sb("gts", [128, 64], F32, MO)
        gT = K.sb("gT", [8, T], BF16, MO)
        rt = {"wr": wr, "banks": []}
        norm_mod(lambda c: gs2b.t[:, c:c + 1], lambda c: mod1.t[:, 24 + c:25 + c], (gs2b, mod1), router=rt)
        for hh in range(2):
            rb = rt["banks"][hh]
            K.op(act, lambda h, hh=hh, rb=rb: h.activation(out=lg.t[:, HS[hh]], in_=rb.t[0:8, :], func=AF.Identity, bias=rbias.t[:, 0:1], scale=1.0),
                 r=(rb, rbias), w=(lg,))
        ltb = K.bank()
        for tt in range(8):
            K.transpose(ltb, ltb.t[:, tt * 8:(tt + 1) * 8], lg.t[0:8, tt * 128:(tt + 1) * 128], ident.t[0:8, 0:8], r=(lg, ident), first=(tt == 0), last=(tt == 7))
        K.op(dve, lambda h: h.tensor_copy(out=lt.t[:], in_=ltb.t[:, 0:64]), r=(ltb,), w=(lt,))
        sm_ = [K.sb(f"rs{i}", [128, 8], F32, MO) for i in range(4)]
        sc_ = [K.sb(f"rc{i}", [128, 1], F32, MO) for i in range(4)]
        for tt in range(8):
            l_ap = lt.t[:, tt * 8:(tt + 1) * 8]
            m1, nm1, m2, ssum = sc_
            ex, eq, l2, sel = sm_
            K.op(dve, lambda h: h.reduce_max(out=m1.t[:], in_=l_ap, axis=mybir.AxisListType.X), r=(lt,), w=(m1,))
            K.op(dve, lambda h: h.tensor_scalar(out=nm1.t[:], in0=m1.t[:], scalar1=-1.0, scalar2=None, op0=ALU.mult), r=(m1,), w=(nm1,))
            K.op(act, lambda h: h.activation(out=ex.t[:], in_=l_ap, func=AF.Exp, bias=nm1.t[:, 0:1], scale=1.0), r=(lt, nm1), w=(ex,))
            K.op(dve, lambda h: h.tensor_scalar(out=eq.t[:], in0=l_ap, scalar1=m1.t[:, 0:1], scalar2=None, op0=ALU.is_equal), r=(lt, m1), w=(eq,))
            K.op(dve, lambda h: h.scalar_tensor_tensor(out=l2.t[:], in0=eq.t[:], scalar=-1e30, op0=ALU.mult, in1=l_ap, op1=ALU.add), r=(eq, lt), w=(l2,))
            K.op(dve, lambda h: h.reduce_max(out=m2.t[:], in_=l2.t[:], axis=mybir.AxisListType.X), r=(l2,), w=(m2,))
            K.op(dve, lambda h: h.tensor_scalar(out=sel.t[:], in0=l_ap, scalar1=m2.t[:, 0:1], scalar2=None, op0=ALU.is_ge), r=(lt, m2), w=(sel,))
            K.op(dve, lambda h: h.tensor_tensor(out=ex.t[:], in0=ex.t[:], in1=sel.t[:], op=ALU.mult), r=(ex, sel), w=(ex,))
            K.op(dve, lambda h: h.reduce_sum(out=ssum.t[:], in_=ex.t[:], axis=mybir.AxisListType.X), r=(ex,), w=(ssum,))
            K.op(dve, lambda h: h.reciprocal(out=ssum.t[:], in_=ssum.t[:]), r=(ssum,), w=(ssum,))
            K.op(dve, lambda h, tt=tt: h.tensor_scalar(out=gts.t[:, tt * 8:(tt + 1) * 8], in0=ex.t[:], scalar1=ssum.t[:, 0:1], scalar2=None, op0=ALU.mult),
                 r=(ex, ssum), w=(gts,))
        for g2 in range(2):
            gb = K.bank()
            for t4 in range(4):
                tt = g2 * 4 + t4
                K.transpose(gb, gb.t[0:8, t4 * 128:(t4 + 1) * 128], gts.t[:, tt * 8:(tt + 1) * 8], ident.t[:], r=(gts, ident), first=(t4 == 0), last=(t4 == 3))
            K.op(act, lambda h, g2=g2, gb=gb: h.activation(out=gT.t[:, HS[g2]], in_=gb.t[0:8, :], func=AF.Copy), r=(gb,), w=(gT,))
        for e in range(8 if stage >= 7 else 0):
            bg = rotbuf("bg", 2, [128, T], BF16, MO)
            for hh in range(2):
                bk = K.bank()
                K.mm(bk, bk.t[:], sel8.t[:, e * 128:(e + 1) * 128], gT.t[:, HS[hh]], r=(sel8, gT), start=True, stop=True)
                K.op(act, lambda h, hh=hh, bk=bk, bg=bg: h.activation(out=bg.t[:, HS[hh]], in_=bk.t[:], func=AF.Copy), r=(bk,), w=(bg,))
            ffn_unit(W["l1_exp_gate"][e], W["l1_exp_up"][e], W["l1_exp_down"][e], 0, 0,
                     lambda oc: mod1.t[:, 40 + oc:41 + oc], mod1, MO, bg=bg)
        K.barrier()
        MO.close()
        K.barrier()
        L1.close()

    FN = contextlib.ExitStack()
    yv = y_d.rearrange("(c p) t -> p c t", p=128)
    yo4 = [K.sb(f"yq{i}", [128, 512], F32, FN) for i in range(4)]
    cnt = [0]
    def fin_half(hh):
        bank = K.bank()
        for c in range(8):
            sq = rotbuf("sq", 3, [128, 512], BF16, FN)
            K.op(act, lambda h, c=c, sq=sq: h.activation(out=sq.t[:], in_=X(c, hh), func=AF.Square), r=(xh[c][hh],), w=(sq,))
            yield
            K.mm(bank, bank.t[:], ones_bf.t[:], sq.t[:], r=(sq, ones_bf), start=(c == 0), stop=(c == 7), finc=True)
            yield
        rstd = rotbuf("rstd", 2, [128, 512], F32, FN)
        K.op(act, lambda h: h.activation(out=rstd.t[:], in_=bank.t[:], func=AF.Ln, bias=EPS, scale=1.0 / D), r=(bank,), w=(rstd,))
        yield
        K.op(act, lambda h: h.activation(out=rstd.t[:], in_=rstd.t[:], func=AF.Exp, scale=-0.5), r=(rstd,), w=(rstd,))
        yield
        for c in range(8):
            t = yo4[cnt[0] % 4]
            cnt[0] += 1
            K.op(dve, lambda h, c=c, t=t: h.scalar_tensor_tensor(out=t.t[:], in0=X(c, hh), scalar=fin_g.t[:, c:c + 1], op0=ALU.mult,
                                                                in1=rstd.t[:], op1=ALU.mult),
                 r=(xh[c][hh], rstd, fin_g), w=(t,))
            yield
            K.dma(sp, yv[:, c, hh * 512:(hh + 1) * 512], t.t[:], t, r=(t,))
            yield
    zipper(fin_half(0), fin_half(1))
    K.barrier()
    FN.close()
    K.finish()
    return nc


def _fm(v, nch):
    return np.ascontiguousarray(np.asarray(v, np.float32).reshape(nch, 128).T)


_CST = {}


def _hy_tables(L):
    t = np.linspace(0.0, 1.0, L, dtype=np.float32)[:, None]
    w = (2.0 * math.pi * np.arange(L, dtype=np.float32)[:, None] / L).astype(np.float32)
    fb = np.linspace(1e-4, 16 - 1, 16, dtype=np.float32)[None, :]
    z = np.concatenate([t, np.cos(fb * w), -np.sin(fb * w)], axis=-1).astype(np.float32)
    max_decay = math.log(1e-2) / 0.3
    min_decay = math.log(1e-2) / 1.5
    deltas = np.linspace(min_decay, max_decay, 512, dtype=np.float32)
    decay = np.exp(-t * np.abs(deltas)).astype(np.float32)
    zin = np.zeros((33, T), np.float32)
    zin[:, :L] = z.T
    dec = np.zeros((T, 512), np.float32)
    dec[:L] = decay
    return zin, dec


def _constants():
    if _CST:
        return _CST
    bf = ml_dtypes.bfloat16
    sel = np.zeros((8, 8, 128), np.float32)
    for e in range(8):
        sel[e, e, :] = 1.0
    _CST["sel8"] = sel.reshape(8, 1024).astype(bf)
    rows = T // 64
    row = np.repeat(np.arange(rows, dtype=np.float32), 64)
    col = np.tile(np.arange(64, dtype=np.float32), rows)
    inv = (10000.0 ** (-np.arange(16, dtype=np.float32) / 16)).astype(np.float32)
    ang = np.concatenate([row[:, None] * inv, col[:, None] * inv], axis=-1).astype(np.float32)
    cos, sin = np.cos(ang).T, np.sin(ang).T
    _CST["ropecos_s"] = np.ascontiguousarray(np.concatenate([cos, cos], 0).astype(np.float32))
    _CST["ropesin_s"] = np.ascontiguousarray(np.concatenate([-sin, sin], 0).astype(np.float32))
    _CST["ropecos_p"] = np.ones((64, T), np.float32)
    _CST["ropesin_p"] = np.zeros((64, T), np.float32)
    _CST["maskb_s"] = np.zeros((128, NKT * 4), np.float32)
    mp = np.full((NKT, 4), NEG, np.float32)
    for kt in range(4, NKT):
        mp[kt, (kt - 4) // 2] = 0.0
    _CST["maskb_p"] = np.ascontiguousarray(np.broadcast_to(mp.reshape(1, NKT * 4), (128, NKT * 4)).astype(np.float32))
    mr = np.zeros((4, T), np.float32)
    for sg in range(4):
        mr[sg, sg * SEG:(sg + 1) * SEG] = 1.0
    _CST["mk_r"] = mr.astype(bf)
    _CST["mk_l_p"] = np.ascontiguousarray(np.repeat(mp.T[:, :, None], 128, axis=2).reshape(4, NKT * 128)).astype(bf)
    _CST["mk_l_s"] = np.zeros((4, NKT * 128), bf)
    _CST["zin_s"], _CST["decay_s"] = _hy_tables(T)
    _CST["zin_p"], _CST["decay_p"] = _hy_tables(SEG)
    N2 = 2 * T
    tt = np.arange(T, dtype=np.float64)[:, None]
    ff = np.arange(T, dtype=np.float64)[None, :]
    ang2 = 2.0 * np.pi * tt * ff / N2
    Fre = np.cos(ang2)
    Fim = -np.sin(ang2)
    Fim[:, 0] = np.cos(np.pi * tt[:, 0])
    Fm = np.zeros((T, N2), np.float64)
    iF = np.zeros((N2, T), np.float64)
    wre = np.full(T, 2.0 / N2)
    wre[0] = 1.0 / N2
    iFre = (np.cos(ang2) * wre[None, :]).T
    iFim = (-np.sin(ang2) * (2.0 / N2)).T
    iFim[0, :] = np.cos(np.pi * tt[:, 0]) / N2
    for c in range(8):
        Fm[:, (2 * c) * 128:(2 * c + 1) * 128] = Fre[:, c * 128:(c + 1) * 128]
        Fm[:, (2 * c + 1) * 128:(2 * c + 2) * 128] = Fim[:, c * 128:(c + 1) * 128]
        iF[(2 * c) * 128:(2 * c + 1) * 128] = iFre[c * 128:(c + 1) * 128]
        iF[(2 * c + 1) * 128:(2 * c + 2) * 128] = iFim[c * 128:(c + 1) * 128]
    _CST["Fm"] = Fm.astype(np.float32).astype(bf)
    iFb = iF.astype(np.float32).astype(bf)
    _CST["iFm_s"] = np.ascontiguousarray(np.broadcast_to(iFb[None], (4, N2, T)))
    ip = np.zeros((4, N2, T), bf)
    for j in range(4):
        ip[j, :, j * SEG:(j + 1) * SEG] = iFb[:, j * SEG:(j + 1) * SEG]
    _CST["iFm_p"] = ip
    return _CST


def prep_inputs(inp):
    f = lambda k: np.asarray(inp[k], np.float32)
    shared = {}
    for L in (0, 1):
        shared[f"l{L}_norm1"] = _fm(f(f"l{L}_norm1"), 8)
        shared[f"l{L}_norm2"] = _fm(f(f"l{L}_norm2"), 8)
        shared[f"l{L}_w_mod"] = f(f"l{L}_w_mod")
        shared[f"l{L}_b_mod"] = _fm(f(f"l{L}_b_mod"), 48)
    shared["l0_w_in"] = f("l0_w_in")
    shared["l0_conv_a"] = np.ascontiguousarray(f("l0_conv_a").reshape(3, 4, 128).transpose(2, 0, 1).reshape(128, 12))
    shared["l0_lru_conv_w"] = np.ascontiguousarray(f("l0_lru_conv_w").reshape(4, 8, 128).transpose(2, 0, 1).reshape(128, 32))
    shared["l0_lru_conv_b"] = _fm(f("l0_lru_conv_b"), 8)
    shared["l0_lru_wa"] = np.ascontiguousarray(f("l0_lru_wa").reshape(16 * 128, 128))
    shared["l0_lru_wi"] = np.ascontiguousarray(f("l0_lru_wi").reshape(16 * 128, 128))
    for k in ("l0_lru_ba", "l0_lru_bi", "l0_lru_lambda"):
        shared[k] = np.ascontiguousarray(f(k).reshape(2, 8, 128).transpose(2, 0, 1).reshape(128, 16))
    shared["l0_w_out"] = f("l0_w_out")
    shared["l0_ffn_gate"] = f("l0_ffn_gate")
    shared["l0_ffn_up"] = f("l0_ffn_up")
    shared["l0_ffn_down"] = f("l0_ffn_down")
    shared["final_norm"] = _fm(f("final_norm"), 8)
    w_in1 = f("l1_w_in")
    shared["l1_w_in"] = w_in1
    shared["l1_w_in_krsw"] = np.ascontiguousarray(np.concatenate([w_in1[:, 672:704], w_in1[:, 640:672]], axis=1))
    shared["l1_q_norm"] = _fm(f("l1_q_norm"), 3)
    shared["l1_kv_norm"] = _fm(f("l1_kv_norm"), 2)
    wq = f("l1_w_q_up").reshape(384, 8, 192)
    shared["l1_wq_nope"] = np.ascontiguousarray(wq[:, :, 0:128].reshape(384, 1024))
    shared["l1_wq_rope"] = np.ascontiguousarray(wq[:, :, 128:192].reshape(384, 512))
    shared["l1_wq_rope_sw"] = np.ascontiguousarray(np.concatenate([wq[:, :, 160:192], wq[:, :, 128:160]], axis=2).reshape(384, 512))
    wkv = f("l1_w_kv_up").reshape(256, 8, 256)
    shared["l1_wk_nope"] = np.ascontiguousarray(wkv[:, :, 0:128].reshape(256, 1024))
    shared["l1_wv"] = np.ascontiguousarray(wkv[:, :, 128:256].reshape(256, 1024))
    shared["l1_hy_short_w"] = np.ascontiguousarray(f("l1_hy_short_w").reshape(3, 12, 128).transpose(2, 0, 1).reshape(128, 36))
    shared["l1_hy_short_b"] = _fm(f("l1_hy_short_b"), 12)
    shared["l1_hy_f_w1"] = f("l1_hy_f_w1")
    shared["l1_hy_f_b1"] = f("l1_hy_f_b1").reshape(64, 1)
    shared["l1_hy_f_w2"] = f("l1_hy_f_w2")
    shared["l1_hy_f_b2"] = f("l1_hy_f_b2").reshape(64, 1)
    shared["l1_hy_f_w3"] = f("l1_hy_f_w3")
    shared["l1_hy_bias"] = _fm(f("l1_hy_bias"), 4)
    shared["l1_w_out"] = f("l1_w_out")
    shared["l1_router_w"] = f("l1_router_w")
    shared["l1_router_b"] = f("l1_router_b").reshape(8, 1)
    shared["l1_exp_gate"] = f("l1_exp_gate")
    shared["l1_exp_up"] = f("l1_exp_up")
    shared["l1_exp_down"] = f("l1_exp_down")
    cst = _constants()
    shared["sel8"] = cst["sel8"]
    shared["mk_r"] = cst["mk_r"]
    shared["Fm"] = cst["Fm"]

    xp, xsm = f("x_prompt"), f("x_sample")
    maps = []
    for core in range(NCORES):
        m = dict(shared)
        if core in (4, 5):
            b = core - 4
            m["xT"] = np.ascontiguousarray(xsm[b].T)
            m["cond"] = _fm(f("c")[b], 8)
            m["h0"] = np.ascontiguousarray(f("state_l0_lru")[b].reshape(2, 8, 128).transpose(2, 0, 1).reshape(128, 16))
            m["ctx_ckvT"] = np.ascontiguousarray(f("cache_l1_ckv")[b].T)
            m["ctx_krT"] = np.ascontiguousarray(f("cache_l1_krope")[b].T)
            for k in ("ropecos", "ropesin", "mk_l", "zin", "decay", "iFm"):
                m[k] = cst[k + "_s"]
            pf = 0.0
        else:
            pc = core if core < 4 else 0
            m["xT"] = np.ascontiguousarray(xp[4 * pc:4 * pc + 4].reshape(T, D).T)
            m["cond"] = _fm(f("c_ctx"), 8)
            m["h0"] = np.zeros((128, 16), np.float32)
            m["ctx_ckvT"] = np.zeros((256, LCTX), np.float32)
            m["ctx_krT"] = np.zeros((64, LCTX), np.float32)
            for k in ("ropecos", "ropesin", "mk_l", "zin", "decay", "iFm"):
                m[k] = cst[k + "_p"]
            pf = 1.0
        fl = np.zeros((128, 4), np.float32)
        fl[:, 0] = pf
        fl[:, 1] = -pf
        fl[:, 2] = 1.0 - pf
        m["flags"] = fl
        maps.append(m)
    return maps


_NC_CACHE = {}


def kernel(**inputs):
    if "nc" not in _NC_CACHE:
        _NC_CACHE["nc"] = build()
    nc = _NC_CACHE["nc"]
    maps = prep_inputs(inputs)
    res = run_bass_kernel_spmd(nc, maps, core_ids=list(range(NCORES)))
    R = res.results
    y_prompt = np.zeros((16, 256, D), np.float32)
    y_sample = np.zeros((2, T, D), np.float32)
    new_lru = np.zeros((16, 2, 1024), np.float32)
    new_ckv = np.zeros((16, 256, 256), np.float32)
    new_kr = np.zeros((16, 256, 64), np.float32)
    for core in range(4):
        new_ckv[4 * core:4 * core + 4] = R[core]["ckv_out"].T.reshape(4, 256, 256)
        new_kr[4 * core:4 * core + 4] = R[core]["kr_out"].T.reshape(4, 256, 64)
        yT = R[core]["yT"]
        y_prompt[4 * core:4 * core + 4] = yT.T.reshape(4, 256, D)
        lo = R[core]["lru_out"].reshape(128, 2, 8, 4)
        new_lru[4 * core:4 * core + 4] = lo.transpose(3, 1, 2, 0).reshape(4, 2, 1024)
    for b in range(2):
        y_sample[b] = R[4 + b]["yT"].T
    return y_prompt, y_sample, new_lru, new_ckv, new_kr
```

```python
import contextlib
import math
import numpy as np
import ml_dtypes
import concourse.bass as bass
import concourse.mybir as mybir
from concourse.bass_utils import run_bass_kernel_spmd

F32 = mybir.dt.float32
BF16 = mybir.dt.bfloat16
I32 = mybir.dt.int32
ALU = mybir.AluOpType
AF = mybir.ActivationFunctionType

T = 1024
D = 1024
SEG = 256
NSEG = 4
EPS = 1e-6
NCORES = 8
LCTX = 512
LK = LCTX + T
NKT = LK // 128
NEG = -30000.0


class Tl:
    def __init__(self, t, name):
        self.t = t
        self.name = name
        self.w = []
        self.r = []
        self.dsem = None
        self.dcnt = 0
        self.psum = False
        self.persistent = False


class Eng:
    def __init__(self, h, sem, name, is_pe=False):
        self.h = h
        self.sem = sem
        self.name = name
        self.n = 0
        self.known = {}
        self.is_pe = is_pe
        self.pending = {}


class KB:
    def __init__(self, nc):
        self.nc = nc
        self.es = contextlib.ExitStack()
        self.sems = {}
        self.nsem = 0
        self.pe = self._eng(nc.tensor, "pe", True)
        self.act = self._eng(nc.scalar, "act")
        self.dve = self._eng(nc.vector, "dve")
        self.pool = self._eng(nc.gpsimd, "pool")
        self.sp = self._eng(nc.sync, "sp")
        self.engs = [self.pe, self.act, self.dve, self.pool, self.sp]
        self.dma_sems = []
        self.uid = 0
        self.banks = [Tl(self.es.enter_context(nc.psum_tensor(f"bank{i}", [128, 512], F32)), f"bank{i}")
                      for i in range(8)]
        for b in self.banks:
            b.psum = True
            b.persistent = True
        self.bank_i = 0
        self.reserved = []

    def _eng(self, h, name, is_pe=False):
        return Eng(h, self.newsem("e_" + name), name, is_pe)

    def newsem(self, name=None):
        self.nsem += 1
        name = name or f"s{self.nsem}"
        s = self.es.enter_context(self.nc.semaphore(name))
        self.sems[name] = s
        return name

    def sb(self, name, shape, dt=F32, stack=None):
        self.uid += 1
        st = stack if stack is not None else self.es
        t = st.enter_context(self.nc.sbuf_tensor(f"{name}_{self.uid}", list(shape), dt))
        tl = Tl(t, name)
        tl.persistent = stack is None
        return tl

    def bank(self):
        while True:
            b = self.banks[self.bank_i]
            self.bank_i = (self.bank_i + 1) % 8
            if b not in self.reserved:
                return b

    def reserve(self):
        b = self.bank()
        self.reserved.append(b)
        return b

    def release(self, b):
        self.reserved.remove(b)

    def _need(self, r, w):
        need = {}
        for t in r:
            for (s, v) in t.w:
                if need.get(s, 0) < v:
                    need[s] = v
            if t.psum:
                for (s, v) in t.r:
                    if need.get(s, 0) < v:
                        need[s] = v
        for t in w:
            for (s, v) in t.w:
                if need.get(s, 0) < v:
                    need[s] = v
            for (s, v) in t.r:
                if need.get(s, 0) < v:
                    need[s] = v
        return need

    def _waits(self, e, need):
        for s, v in need.items():
            if e.is_pe and s == e.sem:
                continue
            if e.known.get(s, 0) >= v:
                continue
            e.h.wait_ge(self.sems[s], v)
            e.known[s] = v

    @staticmethod
    def _addr(t, ev):
        for i, (s, v) in enumerate(t.r):
            if s == ev[0]:
                if v < ev[1]:
                    t.r[i] = ev
                return
        t.r.append(ev)

    def _flush(self, e):
        if e.pending:
            self._waits(e, e.pending)
            e.pending = {}

    def op(self, e, fn, r=(), w=(), inc=True):
        if any(not t.persistent for t in w):
            self._flush(e)
        self._waits(e, self._need(r, w))
        ins = fn(e.h)
        if inc:
            e.n += 1
            ins.then_inc(self.sems[e.sem], 1)
            ev = (e.sem, e.n)
        else:
            ev = (e.sem, e.n + 1)
        for t in w:
            t.w = [ev]
            t.r = []
        for t in r:
            self._addr(t, ev)
        return ins

    def mm(self, bank, out_ap, lhsT, rhs, r, start, stop, first=None, last=None, finc=False):
        first = start if first is None else first
        last = stop if last is None else last
        e = self.pe
        need = self._need(r, (bank,) if first else ())
        self._waits(e, need)
        ins = e.h.matmul(out_ap, lhsT, rhs, start=start, stop=stop)
        if first:
            bank.w = []
            bank.r = []
        if last or finc or stop:
            e.n += 1
            ins.then_inc(self.sems[e.sem], 1)
            ev = (e.sem, e.n)
            if last:
                bank.w = [ev]
        else:
            ev = (e.sem, e.n + 1)
        for t in r:
            self._addr(t, ev)

    def transpose(self, bank, out_ap, in_ap, ident_ap, r, first=True, last=True):
        e = self.pe
        self._waits(e, self._need(r, (bank,) if first else ()))
        ins = e.h.transpose(out_ap, in_ap, ident_ap)
        if first:
            bank.w = []
            bank.r = []
        if last:
            e.n += 1
            ins.then_inc(self.sems[e.sem], 1)
            ev = (e.sem, e.n)
            bank.w = [ev]
        else:
            ev = (e.sem, e.n + 1)
        for t in r:
            self._addr(t, ev)

    def dma(self, q, out, in_, st, r=(), w=(), lazy_ok=False):
        if any(not t.persistent for t in w):
            self._flush(q)
        self._waits(q, self._need(r, w))
        ins = q.h.dma_start(out=out, in_=in_)
        if st.dsem is None:
            st.dsem = {}
        if q.name not in st.dsem:
            st.dsem[q.name] = [self.newsem(), 0]
            self.dma_sems.append(st.dsem[q.name])
        ds = st.dsem[q.name]
        ds[1] += 16
        ins.then_inc(self.sems[ds[0]], 16)
        ev = (ds[0], ds[1])
        for t in w:
            t.w = [ev]
            t.r = []
        for t in r:
            self._addr(t, ev)

    def barrier(self):
        for e in self.engs:
            tgt = {}
            for f in self.engs:
                if f is e or f.n == 0:
                    continue
                tgt[f.sem] = f.n
            for ds in self.dma_sems:
                tgt[ds[0]] = ds[1]
            for s_, v in tgt.items():
                if e.pending.get(s_, 0) < v:
                    e.pending[s_] = v

    def finish(self):
        sp = self.sp
        for f in self.engs:
            if f is sp or f.n == 0:
                continue
            if sp.known.get(f.sem, 0) < f.n:
                sp.h.wait_ge(self.sems[f.sem], f.n)
        for ds in self.dma_sems:
            sp.h.wait_ge(self.sems[ds[0]], ds[1])
        self.es.close()


class Ring:
    def __init__(self, K, n):
        self.K = K
        self.slots = [K.sb(f"ring{i}", [128, 4096], BF16) for i in range(n)]
        self.i = 0

    def load(self, src3, k, n, q=None, hold=False):
        K = self.K
        while True:
            s = self.slots[self.i]
            self.i = (self.i + 1) % len(self.slots)
            if not getattr(s, "held", False):
                break
        s.held = hold
        dst = s.t[:, 0:k * n].rearrange("p (k n) -> p k n", k=k)
        K.dma(q or K.pool, dst, src3, s, w=(s,), lazy_ok=True)
        return s, dst


def wview(w, r0, k, c0, n):
    return w[r0:r0 + 128 * k, c0:c0 + n].rearrange("(k p) n -> p k n", p=128)


def build(stage=99, debug=False):
    nc = bass.Bass("TRN2", target_bir_lowering=False)
    K = KB(nc)
    pe, act, dve, pool, sp = K.pe, K.act, K.dve, K.pool, K.sp

    def din(name, shape, dt=F32):
        return nc.dram_tensor(name, list(shape), dt, kind="ExternalInput").ap()

    def dout(name, shape, dt=F32):
        return nc.dram_tensor(name, list(shape), dt, kind="ExternalOutput").ap()

    xT_d = din("xT", [D, T])
    cond_d = din("cond", [128, 8])
    h0_d = din("h0", [128, 16])
    pflag_d = din("flags", [128, 4])
    y_d = dout("yT", [D, T])
    lru_d = dout("lru_out", [128, 2 * 8 * 4])
    W = {}
    for L in (0, 1):
        W[f"l{L}_norm1"] = din(f"l{L}_norm1", [128, 8])
        W[f"l{L}_norm2"] = din(f"l{L}_norm2", [128, 8])
        W[f"l{L}_w_mod"] = din(f"l{L}_w_mod", [D, 6 * D])
        W[f"l{L}_b_mod"] = din(f"l{L}_b_mod", [128, 48])
    W["l0_w_in"] = din("l0_w_in", [D, 3584])
    W["l0_conv_a"] = din("l0_conv_a", [128, 12])
    W["l0_lru_conv_w"] = din("l0_lru_conv_w", [128, 32])
    W["l0_lru_conv_b"] = din("l0_lru_conv_b", [128, 8])
    W["l0_lru_wa"] = din("l0_lru_wa", [16 * 128, 128])
    W["l0_lru_wi"] = din("l0_lru_wi", [16 * 128, 128])
    W["l0_lru_ba"] = din("l0_lru_ba", [128, 16])
    W["l0_lru_bi"] = din("l0_lru_bi", [128, 16])
    W["l0_lru_lambda"] = din("l0_lru_lambda", [128, 16])
    W["l0_w_out"] = din("l0_w_out", [1536, D])
    W["l0_ffn_gate"] = din("l0_ffn_gate", [D, 2816])
    W["l0_ffn_up"] = din("l0_ffn_up", [D, 2816])
    W["l0_ffn_down"] = din("l0_ffn_down", [2816, D])
    W["final_norm"] = din("final_norm", [128, 8])
    W["l1_w_in"] = din("l1_w_in", [D, 2240])
    W["l1_w_in_krsw"] = din("l1_w_in_krsw", [D, 64])
    W["l1_q_norm"] = din("l1_q_norm", [128, 3])
    W["l1_kv_norm"] = din("l1_kv_norm", [128, 2])
    W["l1_wq_nope"] = din("l1_wq_nope", [384, 1024])
    W["l1_wq_rope"] = din("l1_wq_rope", [384, 512])
    W["l1_wq_rope_sw"] = din("l1_wq_rope_sw", [384, 512])
    W["l1_wk_nope"] = din("l1_wk_nope", [256, 1024])
    W["l1_wv"] = din("l1_wv", [256, 1024])
    W["l1_hy_short_w"] = din("l1_hy_short_w", [128, 36])
    W["l1_hy_short_b"] = din("l1_hy_short_b", [128, 12])
    W["l1_hy_f_w1"] = din("l1_hy_f_w1", [33, 64])
    W["l1_hy_f_b1"] = din("l1_hy_f_b1", [64, 1])
    W["l1_hy_f_w2"] = din("l1_hy_f_w2", [64, 64])
    W["l1_hy_f_b2"] = din("l1_hy_f_b2", [64, 1])
    W["l1_hy_f_w3"] = din("l1_hy_f_w3", [64, 1024])
    W["l1_hy_bias"] = din("l1_hy_bias", [128, 4])
    W["l1_w_out"] = din("l1_w_out", [1536, D])
    W["l1_router_w"] = din("l1_router_w", [D, 8])
    W["l1_router_b"] = din("l1_router_b", [8, 1])
    W["l1_exp_gate"] = din("l1_exp_gate", [8, D, 1408])
    W["l1_exp_up"] = din("l1_exp_up", [8, D, 1408])
    W["l1_exp_down"] = din("l1_exp_down", [8, 1408, D])
    sel8_d = din("sel8", [8, 1024], BF16)
    Fm_d = din("Fm", [T, 2 * T], BF16)
    iFm_d = din("iFm", [4, 2 * T, T], BF16)
    ctx_ckv_d = din("ctx_ckvT", [256, LCTX])
    ctx_kr_d = din("ctx_krT", [64, LCTX])
    ropecos_d = din("ropecos", [64, T])
    ropesin_d = din("ropesin", [64, T])
    mk_l_d = din("mk_l", [4, NKT * 128], BF16)
    mk_r_d = din("mk_r", [4, T], BF16)
    zin_d = din("zin", [33, T])
    decay_d = din("decay", [T, 512])
    ckv_out_d = dout("ckv_out", [256, T])
    kr_out_d = dout("kr_out", [64, T])
    dbg_d = dout("dbg", [8, 128, T]) if debug else None

    ring = Ring(K, 6)
    xs = K.sb("x", [128, 8, T], F32)
    xh = [[Tl(xs.t, f"x{c}_{hh}") for hh in range(2)] for c in range(8)]
    for row in xh:
        for t_ in row:
            t_.persistent = True
    hs = K.sb("h", [128, 8, T], BF16)
    hh_ = [[Tl(hs.t, f"h{c}_{hh}") for hh in range(2)] for c in range(8)]
    for row in hh_:
        for t_ in row:
            t_.persistent = True
    ym = [K.sb(f"ymix{c}", [128, T], BF16) for c in range(12)]
    ones_bf = K.sb("ones_bf", [128, 128], BF16)
    ident = K.sb("ident", [128, 128], F32)
    cond = K.sb("cond", [128, 8], F32)
    siluc = K.sb("siluc", [128, 8], BF16)
    flags = K.sb("flags", [128, 4], F32)
    fin_g = K.sb("fin_g", [128, 8], F32)
    dbg_tile = K.sb("dbgt", [128, T], F32) if debug else None

    def X(c, hh):
        return xs.t[:, c, hh * 512:(hh + 1) * 512]

    def H(c, hh):
        return hs.t[:, c, hh * 512:(hh + 1) * 512]

    def small(name, d_ap, shape, dt=F32, stack=None, q=None):
        t = K.sb(name, shape, dt, stack)
        K.dma(q or sp, t.t[:], d_ap, t, w=(t,))
        return t

    K.op(dve, lambda h: h.memset(ones_bf.t[:], 1.0), w=(ones_bf,))
    K.op(pool, lambda h: h.memset(ident.t[:], 1.0), w=(ident,))
    K.op(pool, lambda h: h.affine_select(out=ident.t[:], in_=ident.t[:], pattern=[[-1, 128]],
                                         compare_op=ALU.is_equal, fill=0.0, base=0, channel_multiplier=1),
         r=(ident,), w=(ident,))
    K.dma(sp, cond.t[:], cond_d, cond, w=(cond,))
    K.dma(sp, flags.t[:], pflag_d, flags, w=(flags,))
    K.dma(sp, fin_g.t[:], W["final_norm"], fin_g, w=(fin_g,))
    xv = xT_d.rearrange("(c p) t -> p c t", p=128)
    for c in range(8):
        K.dma(sp, xs.t[:, c, :], xv[:, c, :], xh[c][0], w=(xh[c][0], xh[c][1]))
    K.op(act, lambda h: h.activation(out=siluc.t[:], in_=cond.t[:], func=AF.Silu), r=(cond,), w=(siluc,))

    rot = {}

    def rotbuf(name, n, shape, dt, stack):
        if not hasattr(stack, "rot"):
            stack.rot = {}
        if name not in stack.rot:
            stack.rot[name] = [[K.sb(f"{name}{i}", shape, dt, stack) for i in range(n)], 0]
        lst = stack.rot[name]
        b = lst[0][lst[1] % n]
        lst[1] += 1
        return b

    bg_tasks = []

    def bg_step(n=1):
        for _ in range(n):
            if bg_tasks:
                bg_tasks.pop(0)()

    def zipper(gF, gB, nf=1, nb=1):
        aF, aB = gF is not None, gB is not None
        while aF or aB:
            for _ in range(nf):
                if aF:
                    try:
                        next(gF)
                    except StopIteration:
                        aF = False
            for _ in range(nb):
                if aB:
                    try:
                        next(gB)
                    except StopIteration:
                        aB = False
    def adaln_alloc(L, stack):
        t = {}
        t["bmod"] = small(f"bmod{L}", W[f"l{L}_b_mod"], [128, 48], stack=stack)
        t["mod"] = K.sb(f"mod{L}", [128, 48], F32, stack)
        t["moda"] = K.sb(f"moda{L}", [128, 16], F32, stack)
        t["n1"] = small(f"n1_{L}", W[f"l{L}_norm1"], [128, 8], stack=stack)
        t["n2"] = small(f"n2_{L}", W[f"l{L}_norm2"], [128, 8], stack=stack)
        t["gs1"] = K.sb(f"gs1_{L}", [128, 8], F32, stack)
        t["gs2"] = K.sb(f"gs2_{L}", [128, 8], F32, stack)
        return t

    def adaln_tasks(L, t):
        wm = W[f"l{L}_w_mod"]
        st = {}
        mod, bmod = t["mod"], t["bmod"]

        moda, modb = t["moda"], t["mod"]

        def step(g):
            if g == 0 or g == 4:
                st["bank"] = K.reserve()
            bank = st["bank"]
            if "next" in st:
                s_, v = st.pop("next")
            else:
                s_, v = ring.load(wview(wm, 0, 8, g * 512, 512), 8, 512, hold=True)
            if g + 1 < 12 and g != 3:
                st["next"] = ring.load(wview(wm, 0, 8, (g + 1) * 512, 512), 8, 512, hold=True)
            for oc in range(4):
                col = g * 4 + oc
                for kc in range(8):
                    K.mm(bank, bank.t[:, col:col + 1], v[:, kc, oc * 128:(oc + 1) * 128], siluc.t[:, kc:kc + 1],
                         r=(s_, siluc), start=(kc == 0), stop=(kc == 7),
                         first=(col in (0, 16) and kc == 0), last=(col in (15, 47) and kc == 7))
            s_.held = False
            if g == 3:
                K.op(dve, lambda h: h.tensor_tensor(out=moda.t[:], in0=bank.t[:, 0:16], in1=bmod.t[:, 0:16], op=ALU.add),
                     r=(bank, bmod), w=(moda,))
                K.release(bank)
                gs, n = t["gs1"], t["n1"]
                K.op(dve, lambda h: h.tensor_tensor(out=gs.t[:], in0=moda.t[:, 8:16], in1=n.t[:], op=ALU.mult), r=(moda, n), w=(gs,))
                K.op(dve, lambda h: h.tensor_tensor(out=gs.t[:], in0=gs.t[:], in1=n.t[:], op=ALU.add), r=(gs, n), w=(gs,))
            if g == 11:
                K.op(dve, lambda h: h.tensor_tensor(out=modb.t[:, 16:48], in0=bank.t[:, 16:48], in1=bmod.t[:, 16:48], op=ALU.add),
                     r=(bank, bmod), w=(modb,))
                K.release(bank)
                gs, n = t["gs2"], t["n2"]
                K.op(dve, lambda h: h.tensor_tensor(out=gs.t[:], in0=modb.t[:, 32:40], in1=n.t[:], op=ALU.mult), r=(modb, n), w=(gs,))
                K.op(dve, lambda h: h.tensor_tensor(out=gs.t[:], in0=gs.t[:], in1=n.t[:], op=ALU.add), r=(gs, n), w=(gs,))
        return [(lambda g=g: step(g)) for g in range(12)]

    def norm_mod(gs_ap, sh_ap, rtiles, stack_unused=None, nfeat=D, out_fn=None, router=None):
        stack = contextlib.ExitStack()

        def half(hh):
            bank = K.bank()
            if router is not None:
                rb = K.bank()
                router["banks"].append(rb)
            for c in range(8):
                sq = rotbuf("sq", 3, [128, 512], BF16, stack)
                K.op(act, lambda h, c=c, sq=sq: h.activation(out=sq.t[:], in_=X(c, hh), func=AF.Square),
                     r=(xh[c][hh],), w=(sq,))
                yield
                K.mm(bank, bank.t[:], ones_bf.t[:], sq.t[:], r=(sq, ones_bf), start=(c == 0), stop=(c == 7), finc=True)
                yield
            rstd = rotbuf("rstd", 2, [128, 512], F32, stack)
            lnv = rotbuf("lnv", 2, [128, 512], F32, stack)
            K.op(act, lambda h: h.activation(out=lnv.t[:], in_=bank.t[:], func=AF.Ln, bias=EPS, scale=1.0 / nfeat),
                 r=(bank,), w=(lnv,))
            yield
            K.op(act, lambda h: h.activation(out=rstd.t[:], in_=lnv.t[:], func=AF.Exp, scale=-0.5),
                 r=(lnv,), w=(rstd,))
            yield
            for c in range(8):
                tmp = rotbuf("nt", 3, [128, 512], F32, stack)
                K.op(dve, lambda h, c=c, tmp=tmp: h.tensor_tensor(out=tmp.t[:], in0=X(c, hh), in1=rstd.t[:], op=ALU.mult),
                     r=(xh[c][hh], rstd), w=(tmp,))
                yield
                if out_fn is None:
                    o_ap, o_t = H(c, hh), hh_[c][hh]
                else:
                    o_ap, o_t = out_fn(c, hh)
                sh = sh_ap(c) if sh_ap is not None else 0.0
                K.op(act, lambda h, c=c, tmp=tmp, o_ap=o_ap, sh=sh: h.activation(
                    out=o_ap, in_=tmp.t[:], func=AF.Identity, bias=sh, scale=gs_ap(c)),
                     r=(tmp,) + tuple(rtiles), w=(o_t,))
                yield
                if router is not None:
                    h2f = rotbuf("h2f", 2, [128, 512], F32, stack)
                    K.op(act, lambda h, c=c, tmp=tmp, h2f=h2f, sh=sh: h.activation(
                        out=h2f.t[:], in_=tmp.t[:], func=AF.Identity, bias=sh, scale=gs_ap(c)),
                         r=(tmp,) + tuple(rtiles), w=(h2f,))
                    yield
                    wr = router["wr"]
                    K.mm(rb, rb.t[0:8, :], wr.t[:, c, :], h2f.t[:], r=(wr, h2f), start=(c == 0), stop=(c == 7), finc=True)
                    yield
        zipper(half(0), half(1))
        K.barrier()
        stack.close()

    def proj(v, s, ncol0, M=128, rhs=None, rt=None, kcs=8):
        banks = []
        for hh in range(2):
            b = K.bank()
            for kc in range(kcs):
                if rhs is None:
                    r_ap, r_t = H(kc, hh), hh_[kc][hh]
                else:
                    r_ap, r_t = rhs(kc, hh), rt(kc, hh)
                K.mm(b, b.t[0:M, :], v[:, kc, ncol0:ncol0 + M], r_ap, r=(s, r_t), start=(kc == 0), stop=(kc == kcs - 1))
            banks.append(b)
        return banks

    HS = [slice(0, 512), slice(512, 1024)]

    def dbg_dump(i, ap, tiles):
        if debug:
            dt_ = dbg_tile
            K.op(dve, lambda h: h.tensor_copy(out=dt_.t[:], in_=ap), r=tuple(tiles), w=(dt_,))
            K.dma(sp, dbg_d[i], dt_.t[:], dt_, r=(dt_,))

    ad1 = adaln_alloc(1, None)
    rS = K.sb("rS", [128, 512], F32)
    L0 = contextlib.ExitStack()
    ad0 = adaln_alloc(0, L0)
    tsk0 = adaln_tasks(0, ad0)
    for tsk in tsk0[:4]:
        tsk()
    bg_tasks.extend(tsk0[4:])
    mod0, moda0, gs1, gs2 = ad0["mod"], ad0["moda"], ad0["gs1"], ad0["gs2"]
    norm_mod(lambda c: gs1.t[:, c:c + 1], lambda c: moda0.t[:, c:c + 1], (gs1, moda0), L0)

    MX = contextlib.ExitStack()
    conva = small("conva", W["l0_conv_a"], [128, 12], stack=MX)
    nconva = K.sb("nconva", [128, 12], F32, MX)
    K.op(dve, lambda h: h.tensor_scalar(out=nconva.t[:], in0=conva.t[:], scalar1=flags.t[:, 1:2], scalar2=None, op0=ALU.mult),
         r=(conva, flags), w=(nconva,))
    lcw = small("lcw", W["l0_lru_conv_w"], [128, 32], stack=MX)
    nlcw = K.sb("nlcw", [128, 32], F32, MX)
    K.op(dve, lambda h: h.tensor_scalar(out=nlcw.t[:], in0=lcw.t[:], scalar1=flags.t[:, 1:2], scalar2=None, op0=ALU.mult),
         r=(lcw, flags), w=(nlcw,))
    lcb = small("lcb", W["l0_lru_conv_b"], [128, 8], stack=MX)
    ba = small("ba", W["l0_lru_ba"], [128, 16], stack=MX)
    bi = small("bi", W["l0_lru_bi"], [128, 16], stack=MX)
    lam = small("lam", W["l0_lru_lambda"], [128, 16], stack=MX)
    h0 = small("h0", h0_d, [128, 16], stack=MX)
    wa = K.sb("wa", [128, 16, 128], BF16, MX)
    wi = K.sb("wi", [128, 16, 128], BF16, MX)
    K.dma(pool, wa.t[:], W["l0_lru_wa"].rearrange("(b h) k -> h b k", h=128), wa, w=(wa,))
    K.dma(pool, wi.t[:], W["l0_lru_wi"].rearrange("(b h) k -> h b k", h=128), wi, w=(wi,))
    lru_out = K.sb("lru_out", [128, 64], F32, MX)
    K.op(pool, lambda h: h.memset(lru_out.t[:], 0.0), w=(lru_out,))
    hba = K.sb("hba", [128, 16], F32, MX)
    hbi = K.sb("hbi", [128, 16], F32, MX)
    K.op(dve, lambda h: h.tensor_scalar(out=hba.t[:], in0=ba.t[:], scalar1=0.5, scalar2=None, op0=ALU.mult), r=(ba,), w=(hba,))
    K.op(dve, lambda h: h.tensor_scalar(out=hbi.t[:], in0=bi.t[:], scalar1=0.5, scalar2=None, op0=ALU.mult), r=(bi,), w=(hbi,))
    spv = K.sb("spv", [128, 16], F32, MX)
    m4 = K.sb("m4", [128, 16], F32, MX)
    m8 = K.sb("m8", [128, 16], F32, MX)
    K.op(act, lambda h: h.activation(out=spv.t[:], in_=lam.t[:], func=AF.Exp, scale=-1.0), r=(lam,), w=(spv,))
    K.op(act, lambda h: h.activation(out=spv.t[:], in_=spv.t[:], func=AF.Ln, bias=1.0, scale=1.0), r=(spv,), w=(spv,))
    K.op(dve, lambda h: h.tensor_scalar(out=m4.t[:], in0=spv.t[:], scalar1=-4.0, scalar2=None, op0=ALU.mult), r=(spv,), w=(m4,))
    K.op(dve, lambda h: h.tensor_scalar(out=m8.t[:], in0=spv.t[:], scalar1=-8.0, scalar2=None, op0=ALU.mult), r=(spv,), w=(m8,))

    w_in0 = W["l0_w_in"]
    pA = [ring.load(wview(w_in0, 0, 8, p * 512, 512), 8, 512, hold=True) for p in range(3)]
    MXA = contextlib.ExitStack()
    for j in range(4 if stage >= 2 else 0):
        bb = proj(pA[0][1], pA[0][0], j * 128)
        cc = proj(pA[1][1], pA[1][0], j * 128)
        xx = proj(pA[2][1], pA[2][0], j * 128)
        U = rotbuf("U3", 2, [128, T + 2], F32, MXA)
        if not getattr(U, "zeroed", False):
            K.op(pool, lambda h, U=U: h.memset(U.t[:], 0.0), w=(U,))
            U.zeroed = True
        tx = rotbuf("tx", 2, [128, T], F32, MXA)
        for hh in range(2):
            K.op(act, lambda h, hh=hh, tx=tx: h.activation(out=tx.t[:, HS[hh]], in_=xx[hh].t[:], func=AF.Copy),
                 r=(xx[hh],), w=(tx,))
            K.op(dve, lambda h, hh=hh, tx=tx, U=U: h.tensor_tensor(out=U.t[:, 1 + hh * 512:1 + (hh + 1) * 512], in0=cc[hh].t[:],
                                                                  in1=tx.t[:, HS[hh]], op=ALU.mult),
                 r=(cc[hh], tx), w=(U,))
        cv = rotbuf("cv", 2, [128, T], F32, MXA)
        K.op(act, lambda h, U=U, cv=cv: h.activation(out=cv.t[:], in_=U.t[:, 0:T], func=AF.Copy, scale=conva.t[:, j:j + 1]),
             r=(U, conva), w=(cv,))
        for k in (1, 2):
            K.op(dve, lambda h, k=k, U=U, cv=cv: h.scalar_tensor_tensor(out=cv.t[:], in0=U.t[:, k:k + T], scalar=conva.t[:, 4 * k + j:4 * k + j + 1],
                                                                      op0=ALU.mult, in1=cv.t[:], op1=ALU.add),
                 r=(U, conva, cv), w=(cv,))
        K.op(dve, lambda h, U=U, cv=cv: h.scalar_tensor_tensor(out=cv.t[:, 256:1024:256], in0=U.t[:, 256:1024:256], scalar=nconva.t[:, j:j + 1],
                                                            op0=ALU.mult, in1=cv.t[:, 256:1024:256], op1=ALU.add),
             r=(U, nconva, cv), w=(cv,))
        K.op(dve, lambda h, U=U, cv=cv: h.scalar_tensor_tensor(out=cv.t[:, 255:1023:256], in0=U.t[:, 257:1025:256], scalar=nconva.t[:, 8 + j:8 + j + 1],
                                                            op0=ALU.mult, in1=cv.t[:, 255:1023:256], op1=ALU.add),
             r=(U, nconva, cv), w=(cv,))
        for hh in range(2):
            K.op(dve, lambda h, hh=hh, cv=cv: h.tensor_tensor(out=ym[j].t[:, HS[hh]], in0=bb[hh].t[:], in1=cv.t[:, HS[hh]], op=ALU.mult),
                 r=(bb[hh], cv), w=(ym[j],))
        bg_step(2)

    for p_ in pA:
        p_[0].held = False
    K.barrier()
    MXA.close()
    pst = {}

    def halves(X):
        if not hasattr(X, "hv"):
            X.hv = [Tl(X.t, X.name + "_h0"), Tl(X.t, X.name + "_h1")]
        return X.hv

    def lru_F(j, out):
        if j % 4 == 0:
            for k_ in ("pG", "pX"):
                if k_ in pst:
                    pst[k_][0].held = False
            pst["pG"] = ring.load(wview(w_in0, 0, 8, 1536 + (j // 4) * 512, 512), 8, 512, hold=True)
            pst["pX"] = ring.load(wview(w_in0, 0, 8, 2560 + (j // 4) * 512, 512), 8, 512, hold=True)
        pG, pX = pst["pG"], pst["pX"]
        gg = proj(pG[1], pG[0], (j % 4) * 128)
        xb = proj(pX[1], pX[0], (j % 4) * 128)
        U = rotbuf("U4", 1, [128, T + 3], F32, MX)
        if not getattr(U, "zeroed", False):
            K.op(pool, lambda h, U=U: h.memset(U.t[:], 0.0), w=(U,))
            yield
            U.zeroed = True
        for hh in range(2):
            K.op(act, lambda h, hh=hh, U=U: h.activation(out=U.t[:, 2 + hh * 512:2 + (hh + 1) * 512], in_=xb[hh].t[:], func=AF.Copy),
                 r=(xb[hh],), w=(U,))
            yield
        gl = rotbuf("gl", 2, [128, T], F32, MX)
        for hh in range(2):
            sq = rotbuf("gsq", 1, [128, 512], F32, MX)
            K.op(act, lambda h, hh=hh, sq=sq: h.activation(out=sq.t[:], in_=gg[hh].t[:], func=AF.Square), r=(gg[hh],), w=(sq,))
            yield
            K.op(dve, lambda h, sq=sq: h.tensor_scalar(out=sq.t[:], in0=sq.t[:], scalar1=0.044715, scalar2=1.0, op0=ALU.mult, op1=ALU.add),
                 r=(sq,), w=(sq,))
            yield
            K.op(dve, lambda h, hh=hh, sq=sq: h.tensor_tensor(out=sq.t[:], in0=sq.t[:], in1=gg[hh].t[:], op=ALU.mult),
                 r=(sq, gg[hh]), w=(sq,))
            yield
            K.op(act, lambda h, sq=sq: h.activation(out=sq.t[:], in_=sq.t[:], func=AF.Tanh, scale=0.7978845608028654), r=(sq,), w=(sq,))
            yield
            K.op(dve, lambda h, hh=hh, sq=sq, gl=gl: h.scalar_tensor_tensor(out=gl.t[:, HS[hh]], in0=sq.t[:], scalar=1.0, op0=ALU.add,
                                                                          in1=gg[hh].t[:], op1=ALU.mult),
                 r=(sq, gg[hh]), w=(gl,))
            yield
        xc = rotbuf("xc", 2, [128, T], F32, MX)
        K.op(act, lambda h, U=U, xc=xc: h.activation(out=xc.t[:], in_=U.t[:, 0:T], func=AF.Identity, bias=lcb.t[:, j:j + 1],
                                                  scale=lcw.t[:, j:j + 1]),
             r=(U, lcw, lcb), w=(xc,))
        yield
        for k in (1, 2, 3):
            K.op(dve, lambda h, k=k, U=U, xc=xc: h.scalar_tensor_tensor(out=xc.t[:], in0=U.t[:, k:k + T], scalar=lcw.t[:, 8 * k + j:8 * k + j + 1],
                                                                      op0=ALU.mult, in1=xc.t[:], op1=ALU.add),
                 r=(U, lcw, xc), w=(xc,))
            yield
        for (ocol, ucol, k) in ((256, 256, 0), (256, 257, 1), (257, 257, 0), (255, 258, 3)):
            K.op(dve, lambda h, ocol=ocol, ucol=ucol, k=k, U=U, xc=xc: h.scalar_tensor_tensor(
                out=xc.t[:, ocol:ocol + 513:256], in0=U.t[:, ucol:ucol + 513:256], scalar=nlcw.t[:, 8 * k + j:8 * k + j + 1],
                op0=ALU.mult, in1=xc.t[:, ocol:ocol + 513:256], op1=ALU.add),
                 r=(U, nlcw, xc), w=(xc,))
            yield
        xcb = rotbuf("xcb", 1, [128, T], BF16, MX)
        K.op(act, lambda h, xc=xc, xcb=xcb: h.activation(out=xcb.t[:], in_=xc.t[:], func=AF.Copy), r=(xc,), w=(xcb,))
        yield
        out.update({"xcb": xcb, "xch": xc, "gl": gl})
        yield

    def lru_G(j, st):
        xcb = st["xcb"]
        thr_ = [rotbuf("thr", 2, [128, T], F32, MX) for _ in range(2)]
        thi_ = [rotbuf("thi", 2, [128, T], F32, MX) for _ in range(2)]
        for d in range(2):
            dj = d * 8 + j
            for (wt, th, hbias) in ((wa, thr_[d], hba), (wi, thi_[d], hbi)):
                for hh in range(2):
                    b = K.bank()
                    K.mm(b, b.t[:], wt.t[:, dj, :], xcb.t[:, HS[hh]], r=(wt, xcb), start=True, stop=True)
                    K.op(act, lambda h, b=b, th=th, hh=hh, hbias=hbias, dj=dj: h.activation(out=th.t[:, HS[hh]], in_=b.t[:], func=AF.Tanh,
                                                                                  bias=hbias.t[:, dj:dj + 1], scale=0.5),
                         r=(b, hbias), w=(halves(th)[hh],))
        st["thr_"], st["thi_"] = thr_, thi_

    def lru_B(j, st):
        xch, gl, thr_, thi_ = st["xch"], st["gl"], st["thr_"], st["thi_"]
        av_ = [rotbuf("av", 2, [128, T], F32, MX) for _ in range(2)]
        a2_ = [rotbuf("a2", 2, [128, T], F32, MX) for _ in range(2)]
        hd_l = [rotbuf("hd", 2, [128, T], F32, MX) for _ in range(2)]
        for d in range(2):
            dj = d * 8 + j
            thr, av, a2 = thr_[d], av_[d], a2_[d]
            for hh in range(2):
                K.op(act, lambda h, thr=thr, av=av, dj=dj, hh=hh: h.activation(out=av.t[:, HS[hh]], in_=thr.t[:, HS[hh]], func=AF.Exp,
                                                                            bias=m4.t[:, dj:dj + 1], scale=m4.t[:, dj:dj + 1]),
                     r=(halves(thr)[hh], m4), w=(halves(av)[hh],))
                yield
                K.op(dve, lambda h, av=av, a2=a2, hh=hh: h.scalar_tensor_tensor(out=a2.t[:, HS[hh]], in0=av.t[:, HS[hh]], scalar=-1.0, op0=ALU.mult,
                                                                              in1=av.t[:, HS[hh]], op1=ALU.mult),
                     r=(halves(av)[hh],), w=(halves(a2)[hh],))
                yield
        for d in range(2):
            a2 = a2_[d]
            for hh in range(2):
                K.op(dve, lambda h, a2=a2, hh=hh: h.tensor_scalar(out=a2.t[:, HS[hh]], in0=a2.t[:, HS[hh]], scalar1=1.0, scalar2=1e-30, op0=ALU.add, op1=ALU.max),
                     r=(halves(a2)[hh],), w=(halves(a2)[hh],))
                yield
        for d in range(2):
            a2 = a2_[d]
            for hh in range(2):
                K.op(act, lambda h, a2=a2, hh=hh: h.activation(out=a2.t[:, HS[hh]], in_=a2.t[:, HS[hh]], func=AF.Ln), r=(halves(a2)[hh],), w=(halves(a2)[hh],))
                yield
                K.op(act, lambda h, a2=a2, hh=hh: h.activation(out=a2.t[:, HS[hh]], in_=a2.t[:, HS[hh]], func=AF.Exp, bias=float(math.log(0.5)), scale=0.5), r=(halves(a2)[hh],), w=(halves(a2)[hh],))
                yield
        for d in range(2):
            thi, av, a2 = thi_[d], av_[d], a2_[d]
            for hh in range(2):
                K.op(dve, lambda h, a2=a2, hh=hh: h.tensor_tensor(out=a2.t[:, HS[hh]], in0=a2.t[:, HS[hh]], in1=xch.t[:, HS[hh]], op=ALU.mult),
                     r=(halves(a2)[hh], xch), w=(halves(a2)[hh],))
                yield
                K.op(dve, lambda h, a2=a2, thi=thi, hh=hh: h.scalar_tensor_tensor(out=a2.t[:, HS[hh]], in0=thi.t[:, HS[hh]], scalar=1.0, op0=ALU.add,
                                                                               in1=a2.t[:, HS[hh]], op1=ALU.mult),
                     r=(halves(a2)[hh], halves(thi)[hh]), w=(halves(a2)[hh],))
                yield
            for (hh, sl) in (((0, slice(256, 257)), (1, slice(512, 769, 256))) if d == 0 else ((0, slice(255, 512, 256)), (1, slice(767, 768)))):
                K.op(dve, lambda h, av=av, sl=sl: h.tensor_scalar(out=av.t[:, sl], in0=av.t[:, sl], scalar1=flags.t[:, 2:3], scalar2=None, op0=ALU.mult),
                     r=(halves(av)[hh], flags), w=(halves(av)[hh],))
                yield
        hdir = []
        for d in range(2):
            dj = d * 8 + j
            av, a2, hd = av_[d], a2_[d], hd_l[d]
            if d == 0:
                K.op(dve, lambda h, av=av, a2=a2, hd=hd, dj=dj: h.tensor_tensor_scan(out=hd.t[:, 0:512], data0=av.t[:, 0:512], data1=a2.t[:, 0:512],
                                                                                 initial=h0.t[:, dj:dj + 1], op0=ALU.mult, op1=ALU.add),
                     r=(halves(av)[0], halves(a2)[0], h0), w=(halves(hd)[0],))
                yield
                K.op(dve, lambda h, av=av, a2=a2, hd=hd: h.tensor_tensor_scan(out=hd.t[:, 512:1024], data0=av.t[:, 512:1024], data1=a2.t[:, 512:1024],
                                                                         initial=hd.t[:, 511:512], op0=ALU.mult, op1=ALU.add),
                     r=(halves(av)[1], halves(a2)[1], halves(hd)[0]), w=(halves(hd)[1],))
                yield
                K.op(pool, lambda h, hd=hd: h.tensor_copy(out=lru_out.t[:, j * 4:(j + 1) * 4], in_=hd.t[:, 255:1024:256]), r=(halves(hd)[0], halves(hd)[1]), w=(lru_out,))
                yield
            else:
                K.op(dve, lambda h, av=av, a2=a2, hd=hd, dj=dj: h.tensor_tensor_scan(out=hd.t[:, 1023:511:-1], data0=av.t[:, 1023:511:-1], data1=a2.t[:, 1023:511:-1],
                                                                                 initial=h0.t[:, dj:dj + 1], op0=ALU.mult, op1=ALU.add),
                     r=(halves(av)[1], halves(a2)[1], h0), w=(halves(hd)[1],))
                yield
                K.op(dve, lambda h, av=av, a2=a2, hd=hd: h.tensor_tensor_scan(out=hd.t[:, 511::-1], data0=av.t[:, 511::-1], data1=a2.t[:, 511::-1],
                                                                         initial=hd.t[:, 512:513], op0=ALU.mult, op1=ALU.add),
                     r=(halves(av)[0], halves(a2)[0], halves(hd)[1]), w=(halves(hd)[0],))
                yield
                K.op(pool, lambda h, hd=hd: h.tensor_copy(out=lru_out.t[:, 32 + j * 4:32 + (j + 1) * 4], in_=hd.t[:, 0:1024:256]), r=(halves(hd)[0], halves(hd)[1]), w=(lru_out,))
                yield
            hdir.append(hd)
        hf, hb = hdir
        for hh in range(2):
            K.op(dve, lambda h, hh=hh: h.tensor_tensor(out=hf.t[:, HS[hh]], in0=hf.t[:, HS[hh]], in1=hb.t[:, HS[hh]], op=ALU.add),
                 r=(halves(hf)[hh], halves(hb)[hh]), w=(halves(hf)[hh],))
            yield
            K.op(dve, lambda h, gl=gl, hh=hh: h.scalar_tensor_tensor(out=ym[4 + j].t[:, HS[hh]], in0=hf.t[:, HS[hh]], scalar=0.5, op0=ALU.mult, in1=gl.t[:, HS[hh]], op1=ALU.mult),
                 r=(halves(hf)[hh], gl), w=(ym[4 + j],))
            yield
    NJ = 8 if stage >= 3 else 0

    if NJ:
        stj = {}
        zipper(lru_F(0, stj), None)
        lru_G(0, stj)
        for j in range(NJ):
            nst = {} if j + 1 < NJ else None
            zipper(lru_F(j + 1, nst) if nst is not None else None, lru_B(j, stj))
            if nst is not None:
                lru_G(j + 1, nst)
            stj = nst
        for k_ in ("pG", "pX"):
            pst[k_][0].held = False
    K.dma(sp, lru_d, lru_out.t[:], lru_out, r=(lru_out,))
    K.barrier()
    MX.close()

    def out_proj(w_out, gate_ap, gate_t):
        for g in range(4):
            s, v = ring.load(wview(w_out, 0, 12, g * 256, 256), 12, 256)
            for o2 in range(2):
                oc = g * 2 + o2
                for hh in range(2):
                    b = K.bank()
                    for kc in range(12):
                        K.mm(b, b.t[:], v[:, kc, o2 * 128:(o2 + 1) * 128], ym[kc].t[:, HS[hh]], r=(s, ym[kc]), start=(kc == 0), stop=(kc == 11))
                    K.op(dve, lambda h, b=b, oc=oc, hh=hh: h.scalar_tensor_tensor(out=X(oc, hh), in0=b.t[:], scalar=gate_ap(oc), op0=ALU.mult,
                                                                               in1=X(oc, hh), op1=ALU.add),
                         r=(b, gate_t, xh[oc][hh]), w=(xh[oc][hh],))

    bg_step(1000)
    if stage >= 4:
        out_proj(W["l0_w_out"], lambda oc: mod0.t[:, 16 + oc:17 + oc], mod0)
    if debug:
        for c in range(2):
            dbg_dump(c, xs.t[:, c, :], (xh[c][0], xh[c][1]))

    def HFB(part, pc):
        return ym[part * 4 + pc // 2].t[:, (pc % 2) * 512:(pc % 2 + 1) * 512], ym[part * 4 + pc // 2]

    def hy_filter_tasks(stack):
        zin = small("zin", zin_d, [33, T], stack=stack)
        fw1 = small("fw1", W["l1_hy_f_w1"], [33, 64], stack=stack)
        fw2 = small("fw2", W["l1_hy_f_w2"], [64, 64], stack=stack)
        fw3 = small("fw3", W["l1_hy_f_w3"], [64, 1024], stack=stack)
        fb1 = small("fb1", W["l1_hy_f_b1"], [64, 1], stack=stack)
        fb2 = small("fb2", W["l1_hy_f_b2"], [64, 1], stack=stack)
        hid = [K.sb(f"hid{i}", [64, T], F32, stack) for i in range(2)]
        ta = K.sb("sin_a", [64, T], F32, stack)
        tb = K.sb("sin_b", [64, T], F32, stack)
        ti = K.sb("sin_i", [64, T], I32, stack)
        dv = decay_d.rearrange("(c p) n -> p c n", p=128)
        st = {"nmm": 0}
        tasks = []
        for li, (wt, bt, src, kdim) in enumerate(((fw1, fb1, zin, 33), (fw2, fb2, hid[0], 64))):
            def mlp_a(wt=wt, bt=bt, src=src, kdim=kdim):
                for hh in range(2):
                    bk = K.bank()
                    K.mm(bk, bk.t[0:64, :], wt.t[0:kdim, :], src.t[0:kdim, HS[hh]], r=(wt, src), start=True, stop=True)
                    K.op(act, lambda h, hh=hh, bk=bk: h.activation(out=ta.t[:, HS[hh]], in_=bk.t[0:64, :], func=AF.Identity, bias=bt.t[:, 0:1], scale=1.0),
                         r=(bk, bt), w=(ta,))

            def mlp_b(li=li):
                K.op(dve, lambda h: h.tensor_scalar(out=ta.t[:], in0=ta.t[:], scalar1=float(1.0 / (2 * math.pi)), scalar2=64.5, op0=ALU.mult, op1=ALU.add), r=(ta,), w=(ta,))
                K.op(dve, lambda h: h.tensor_copy(out=ti.t[:], in_=ta.t[:]), r=(ta,), w=(ti,))
                K.op(dve, lambda h: h.tensor_copy(out=tb.t[:], in_=ti.t[:]), r=(ti,), w=(tb,))
                K.op(dve, lambda h: h.tensor_tensor(out=ta.t[:], in0=ta.t[:], in1=tb.t[:], op=ALU.subtract), r=(ta, tb), w=(ta,))
                K.op(dve, lambda h: h.tensor_scalar(out=tb.t[:], in0=ta.t[:], scalar1=0.0, scalar2=None, op0=ALU.is_lt), r=(ta,), w=(tb,))
                K.op(dve, lambda h: h.tensor_tensor(out=ta.t[:], in0=ta.t[:], in1=tb.t[:], op=ALU.add), r=(ta, tb), w=(ta,))
                K.op(act, lambda h: h.activation(out=hid[li].t[:], in_=ta.t[:], func=AF.Sin, bias=float(-math.pi), scale=float(2 * math.pi)), r=(ta,), w=(hid[li],))
            tasks += [mlp_a, mlp_b]

        def hfil(pc):
            if pc == 0:
                st["sbank"] = K.reserve()
            sbank = st["sbank"]
            dec = rotbuf("dec", 2, [128, 512], F32, stack)
            K.dma(sp, dec.t[:], dv[:, pc, :], dec, w=(dec,))
            for part in range(2):
                bk = K.bank()
                K.mm(bk, bk.t[:], hid[1].t[:, pc * 128:(pc + 1) * 128], fw3.t[:, part * 512:(part + 1) * 512], r=(hid[1], fw3), start=True, stop=True)
                tmp = rotbuf("hft", 2, [128, 512], F32, stack)
                K.op(dve, lambda h, bk=bk, tmp=tmp, dec=dec: h.tensor_tensor(out=tmp.t[:], in0=bk.t[:], in1=dec.t[:], op=ALU.mult), r=(bk, dec), w=(tmp,))
                if part == 1 and pc == 0:
                    K.op(dve, lambda h, tmp=tmp: h.memset(tmp.t[0:1, :], 0.0), r=(tmp,), w=(tmp,))
                o_ap, o_t = HFB(part, pc)
                K.op(act, lambda h, tmp=tmp, o_ap=o_ap: h.activation(out=o_ap, in_=tmp.t[:], func=AF.Copy), r=(tmp,), w=(o_t,))
                ab = rotbuf("hfab", 2, [128, 512], BF16, stack)
                K.op(act, lambda h, tmp=tmp, ab=ab: h.activation(out=ab.t[:], in_=tmp.t[:], func=AF.Abs), r=(tmp,), w=(ab,))
                K.mm(sbank, sbank.t[:], ones_bf.t[:], ab.t[:], r=(ab, ones_bf), start=(st["nmm"] == 0), stop=(st["nmm"] == 15), finc=True)
                st["nmm"] += 1
        tasks += [(lambda pc=pc: hfil(pc)) for pc in range(8)]

        def fin():
            sbank = st["sbank"]
            K.op(dve, lambda h: h.reciprocal(out=rS.t[:], in_=sbank.t[:]), r=(sbank,), w=(rS,))
            K.release(sbank)
        tasks.append(fin)
        return tasks

    def ffn_unit(Wg, Wu, Wd, gcol0, drow0, gate_ap, gate_t, stack, bg=None):
        actt = [rotbuf("actt", 11, [128, T], BF16, stack) for _ in range(11)]
        jj = 0
        for (c0, n) in ((0, 512), (512, 512), (1024, 384)):
            sg_, vg = ring.load(wview(Wg, 0, 8, gcol0 + c0, n), 8, n, hold=True)
            su_, vu = ring.load(wview(Wu, 0, 8, gcol0 + c0, n), 8, n, hold=True)
            for jc in range(n // 128):
                bgk = proj(vg, sg_, jc * 128)
                buk = proj(vu, su_, jc * 128)
                for hh in range(2):
                    sgt = rotbuf("sgt", 3, [128, 512], F32, stack)
                    K.op(act, lambda h, hh=hh, sgt=sgt: h.activation(out=sgt.t[:], in_=bgk[hh].t[:], func=AF.Silu), r=(bgk[hh],), w=(sgt,))
                    if bg is None:
                        K.op(dve, lambda h, hh=hh, sgt=sgt, jj=jj: h.tensor_tensor(out=actt[jj].t[:, HS[hh]], in0=sgt.t[:], in1=buk[hh].t[:], op=ALU.mult),
                             r=(sgt, buk[hh]), w=(actt[jj],))
                    else:
                        K.op(dve, lambda h, hh=hh, sgt=sgt: h.tensor_tensor(out=sgt.t[:], in0=sgt.t[:], in1=buk[hh].t[:], op=ALU.mult),
                             r=(sgt, buk[hh]), w=(sgt,))
                        K.op(dve, lambda h, hh=hh, sgt=sgt, jj=jj: h.tensor_tensor(out=actt[jj].t[:, HS[hh]], in0=sgt.t[:], in1=bg.t[:, HS[hh]], op=ALU.mult),
                             r=(sgt, bg), w=(actt[jj],))
                jj += 1
                if jc == n // 128 - 1:
                    sg_.held = False
                    su_.held = False
                bg_step()
        for g in range(4):
            s, v = ring.load(wview(Wd, drow0, 11, g * 256, 256), 11, 256)
            for o2 in range(2):
                oc = g * 2 + o2
                for hh in range(2):
                    b = K.bank()
                    for kc in range(11):
                        K.mm(b, b.t[:], v[:, kc, o2 * 128:(o2 + 1) * 128], actt[kc].t[:, HS[hh]], r=(s, actt[kc]), start=(kc == 0), stop=(kc == 10))
                    K.op(dve, lambda h, b=b, oc=oc, hh=hh: h.scalar_tensor_tensor(out=X(oc, hh), in0=b.t[:], scalar=gate_ap(oc), op0=ALU.mult,
                                                                               in1=X(oc, hh), op1=ALU.add),
                         r=(b, gate_t, xh[oc][hh]), w=(xh[oc][hh],))

    norm_mod(lambda c: gs2.t[:, c:c + 1], lambda c: mod0.t[:, 24 + c:25 + c], (gs2, mod0), L0)
    FF = contextlib.ExitStack()
    if stage >= 6:
        bg_tasks.extend(hy_filter_tasks(FF))
        bg_tasks.extend(adaln_tasks(1, ad1))
    for u in range(2 if stage >= 5 else 0):
        ffn_unit(W["l0_ffn_gate"], W["l0_ffn_up"], W["l0_ffn_down"], 1408 * u, 1408 * u,
                 lambda oc: mod0.t[:, 40 + oc:41 + oc], mod0, FF)
    bg_step(1000)
    K.barrier()
    FF.close()
    L0.close()
    if debug:
        for c in range(2):
            dbg_dump(2 + c, xs.t[:, c, :], (xh[c][0], xh[c][1]))


    if stage >= 6:
        L1 = contextlib.ExitStack()
        bg_step(1000)
        mod1, moda1, gs1b, gs2b = ad1["mod"], ad1["moda"], ad1["gs1"], ad1["gs2"]
        norm_mod(lambda c: gs1b.t[:, c:c + 1], lambda c: moda1.t[:, c:c + 1], (gs1b, moda1))
        SC = 1.0 / math.sqrt(192.0)
        cqn = [K.sb(f"cqn{c}", [128, T], BF16, L1) for c in range(3)]
        ckv_all = [K.sb(f"ckva{c}", [128, LK], BF16, L1) for c in range(2)]
        kr_all = K.sb("kr_all", [128, LK], BF16, L1)
        K.op(pool, lambda h: h.memset(kr_all.t[64:128, :], 0.0), w=(kr_all,))
        K.dma(sp, kr_all.t[64:68, :], mk_l_d, kr_all, r=(kr_all,), w=(kr_all,))
        w_in1 = W["l1_w_in"]

        def small_norm(src, n, nfeat, gain, emit, stack):
            def half(hh):
                    bank = K.bank()
                    for c in range(n):
                        sq = rotbuf("sq", 3, [128, 512], BF16, stack)
                        K.op(act, lambda h, c=c, sq=sq: h.activation(out=sq.t[:], in_=src[c].t[:, HS[hh]], func=AF.Square), r=(src[c],), w=(sq,))
                        yield
                        K.mm(bank, bank.t[:], ones_bf.t[:], sq.t[:], r=(sq, ones_bf), start=(c == 0), stop=(c == n - 1), finc=True)
                        yield
                    rstd = rotbuf("rstd", 2, [128, 512], F32, stack)
                    K.op(act, lambda h: h.activation(out=rstd.t[:], in_=bank.t[:], func=AF.Ln, bias=EPS, scale=1.0 / nfeat), r=(bank,), w=(rstd,))
                    yield
                    K.op(act, lambda h: h.activation(out=rstd.t[:], in_=rstd.t[:], func=AF.Exp, scale=-0.5), r=(rstd,), w=(rstd,))
                    yield
                    for c in range(n):
                        tmp = rotbuf("nt", 3, [128, 512], F32, stack)
                        K.op(dve, lambda h, c=c, tmp=tmp: h.tensor_tensor(out=tmp.t[:], in0=src[c].t[:, HS[hh]], in1=rstd.t[:], op=ALU.mult),
                             r=(src[c], rstd), w=(tmp,))
                        yield
                        emit(c, hh, tmp, gain)
                        yield
            zipper(half(0), half(1))

        P1 = contextlib.ExitStack()
        qn_g = small("qn_g", W["l1_q_norm"], [128, 3], stack=P1)
        kvn_g = small("kvn_g", W["l1_kv_norm"], [128, 2], stack=P1)
        rcos = small("rcos", ropecos_d, [64, T], stack=P1)
        rsin = small("rsin", ropesin_d, [64, T], stack=P1)
        cq = [K.sb(f"cq{c}", [128, T], F32, P1) for c in range(3)]
        ckr = [K.sb(f"ckr{c}", [128, T], F32, P1) for c in range(2)]
        ckvn = K.sb("ckvn", [128, 2, T], F32, P1)
        krf = K.sb("krf", [64, T], F32, P1)
        ctxf = K.sb("ctxf", [128, 2, LCTX], F32, P1)
        ctxk = K.sb("ctxk", [64, LCTX], F32, P1)
        K.dma(sp, ctxf.t[:], ctx_ckv_d.rearrange("(c p) t -> p c t", p=128), ctxf, w=(ctxf,))
        K.dma(sp, ctxk.t[:], ctx_kr_d, ctxk, w=(ctxk,))
        for c in range(2):
            K.op(act, lambda h, c=c: h.activation(out=ckv_all[c].t[:, 0:LCTX], in_=ctxf.t[:, c, :], func=AF.Copy), r=(ctxf,), w=(ckv_all[c],))
        K.op(act, lambda h: h.activation(out=kr_all.t[0:64, 0:LCTX], in_=ctxk.t[:], func=AF.Copy), r=(ctxk,), w=(kr_all,))
        pa = ring.load(wview(w_in1, 0, 8, 0, 512), 8, 512)
        pb = ring.load(wview(w_in1, 0, 8, 512, 192), 8, 192)
        pc_ = ring.load(wview(W["l1_w_in_krsw"], 0, 8, 0, 64), 8, 64)
        for c in range(3):
            bk = proj(pa[1], pa[0], c * 128)
            for hh in range(2):
                K.op(act, lambda h, c=c, hh=hh: h.activation(out=cq[c].t[:, HS[hh]], in_=bk[hh].t[:], func=AF.Copy), r=(bk[hh],), w=(cq[c],))
        for c in range(2):
            bk = proj(pa[1], pa[0], 384) if c == 0 else proj(pb[1], pb[0], 0)
            for hh in range(2):
                K.op(act, lambda h, c=c, hh=hh: h.activation(out=ckr[c].t[:, HS[hh]], in_=bk[hh].t[:], func=AF.Copy), r=(bk[hh],), w=(ckr[c],))
        bkr = proj(pb[1], pb[0], 128, M=64)
        bks = proj(pc_[1], pc_[0], 0, M=64)
        for hh in range(2):
            K.op(act, lambda h, hh=hh: h.activation(out=krf.t[:, HS[hh]], in_=bkr[hh].t[0:64, :], func=AF.Copy), r=(bkr[hh],), w=(krf,))
            t1 = rotbuf("rt1", 1, [64, 512], F32, P1)
            t2 = rotbuf("rt2", 1, [64, 512], F32, P1)
            K.op(dve, lambda h, hh=hh, t1=t1: h.tensor_tensor(out=t1.t[:], in0=bkr[hh].t[0:64, :], in1=rcos.t[:, HS[hh]], op=ALU.mult), r=(bkr[hh], rcos), w=(t1,))
            K.op(dve, lambda h, hh=hh, t2=t2: h.tensor_tensor(out=t2.t[:], in0=bks[hh].t[0:64, :], in1=rsin.t[:, HS[hh]], op=ALU.mult), r=(bks[hh], rsin), w=(t2,))
            K.op(dve, lambda h, hh=hh, t1=t1, t2=t2: h.tensor_tensor(out=kr_all.t[0:64, LCTX + hh * 512:LCTX + (hh + 1) * 512], in0=t1.t[:], in1=t2.t[:], op=ALU.add),
                 r=(t1, t2), w=(kr_all,))
        K.dma(sp, kr_out_d, krf.t[:], krf, r=(krf,))

        def emit_cq(c, hh, tmp, gain):
            K.op(act, lambda h: h.activation(out=cqn[c].t[:, HS[hh]], in_=tmp.t[:], func=AF.Copy, scale=gain.t[:, c:c + 1]), r=(tmp, gain), w=(cqn[c],))

        def emit_ckv(c, hh, tmp, gain):
            K.op(act, lambda h: h.activation(out=ckvn.t[:, c, HS[hh]], in_=tmp.t[:], func=AF.Copy, scale=gain.t[:, c:c + 1]), r=(tmp, gain), w=(ckvn,))
            K.op(act, lambda h: h.activation(out=ckv_all[c].t[:, LCTX + hh * 512:LCTX + (hh + 1) * 512], in_=tmp.t[:], func=AF.Copy, scale=gain.t[:, c:c + 1]),
                 r=(tmp, gain), w=(ckv_all[c],))
        small_norm(cq, 3, 384, qn_g, emit_cq, P1)
        small_norm(ckr, 2, 256, kvn_g, emit_ckv, P1)
        K.dma(sp, ckv_out_d.rearrange("(c p) t -> p c t", p=128), ckvn.t[:], ckvn, r=(ckvn,))
        K.barrier()
        P1.close()

        P2 = contextlib.ExitStack()
        hsw = small("hsw", W["l1_hy_short_w"], [128, 36], stack=P2)
        nhsw = K.sb("nhsw", [128, 36], F32, P2)
        K.op(dve, lambda h: h.tensor_scalar(out=nhsw.t[:], in0=hsw.t[:], scalar1=flags.t[:, 1:2], scalar2=None, op0=ALU.mult), r=(hsw, flags), w=(nhsw,))
        hsb = small("hsb", W["l1_hy_short_b"], [128, 12], stack=P2)
        zf = [K.sb(f"zf{j}", [128, T], F32, P2) for j in range(4)]
        pu = [ring.load(wview(w_in1, 0, 8, 704 + p * 512, 512), 8, 512) for p in range(3)]

        def uh_conv(c, out_ap, out_tile, mul_tile=None):
            bk = proj(pu[c // 4][1], pu[c // 4][0], (c % 4) * 128)
            U = rotbuf("Uh", 2, [128, T + 2], F32, P2)
            if not getattr(U, "zeroed", False):
                K.op(pool, lambda h, U=U: h.memset(U.t[:], 0.0), w=(U,))
                yield
                U.zeroed = True
            for hh in range(2):
                K.op(act, lambda h, hh=hh: h.activation(out=U.t[:, 1 + hh * 512:1 + (hh + 1) * 512], in_=bk[hh].t[:], func=AF.Copy), r=(bk[hh],), w=(U,))
                yield
            cv = rotbuf("cvh", 2, [128, T], F32, P2)
            K.op(act, lambda h: h.activation(out=cv.t[:], in_=U.t[:, 0:T], func=AF.Identity, bias=hsb.t[:, c:c + 1], scale=hsw.t[:, c:c + 1]),
                 r=(U, hsw, hsb), w=(cv,))
            yield
            for k in (1, 2):
                K.op(dve, lambda h, k=k: h.scalar_tensor_tensor(out=cv.t[:], in0=U.t[:, k:k + T], scalar=hsw.t[:, 12 * k + c:12 * k + c + 1],
                                                             op0=ALU.mult, in1=cv.t[:], op1=ALU.add), r=(U, hsw, cv), w=(cv,))
                yield
            K.op(dve, lambda h: h.scalar_tensor_tensor(out=cv.t[:, 256:1024:256], in0=U.t[:, 256:1024:256], scalar=nhsw.t[:, c:c + 1],
                                                    op0=ALU.mult, in1=cv.t[:, 256:1024:256], op1=ALU.add), r=(U, nhsw, cv), w=(cv,))
            yield
            K.op(dve, lambda h: h.scalar_tensor_tensor(out=cv.t[:, 255:1023:256], in0=U.t[:, 257:1025:256], scalar=nhsw.t[:, 24 + c:24 + c + 1],
                                                    op0=ALU.mult, in1=cv.t[:, 255:1023:256], op1=ALU.add), r=(U, nhsw, cv), w=(cv,))
            yield
            if mul_tile is None:
                K.op(act, lambda h: h.activation(out=out_ap, in_=cv.t[:], func=AF.Copy), r=(cv,), w=(out_tile,))
                yield
            else:
                K.op(dve, lambda h: h.tensor_tensor(out=out_ap, in0=cv.t[:], in1=mul_tile.t[:], op=ALU.mult), r=(cv, mul_tile), w=(out_tile,))
                yield

        def zchain(j):
            yield from uh_conv(4 + j, zf[j].t[:], zf[j])
            yield from uh_conv(8 + j, zf[j].t[:], zf[j], mul_tile=zf[j])
        for j in (0, 2):
            zipper(uh_conv(j, ym[8 + j].t[:], ym[8 + j]), uh_conv(j + 1, ym[9 + j].t[:], ym[9 + j]))
        for j in (0, 2):
            zipper(zchain(j), zchain(j + 1))
        for j in range(4):
            K.op(act, lambda h, j=j: h.activation(out=hs.t[:, j, :], in_=zf[j].t[:], func=AF.Copy), r=(zf[j],), w=(hh_[j][0], hh_[j][1]))
        for tc in range(8):
            bk = K.bank()
            for j in range(4):
                K.transpose(bk, bk.t[:, j * 128:(j + 1) * 128], zf[j].t[:, tc * 128:(tc + 1) * 128], ident.t[:], r=(zf[j], ident),
                            first=(j == 0), last=(j == 3))
            K.op(act, lambda h, tc=tc, bk=bk: h.activation(out=hs.t[:, 4 + tc // 2, (tc % 2) * 512:(tc % 2 + 1) * 512], in_=bk.t[:], func=AF.Copy),
                 r=(bk,), w=(hh_[4 + tc // 2][tc % 2],))
        K.barrier()
        P2.close()

        def ZTM(tc):
            return hs.t[:, 4 + tc // 2, (tc % 2) * 512:(tc % 2 + 1) * 512], hh_[4 + tc // 2][tc % 2]

        P3 = contextlib.ExitStack()
        Kt = [K.sb(f"Kq{q}", [128, 512], F32, P3) for q in range(4)]
        Ys = [[K.sb(f"Ys{j}_{q}", [128, 512], BF16, P3) for q in range(4)] for j in range(4)]
        yacc = [K.sb(f"yacc{j}", [128, T], F32, P3) for j in range(4)]
        hyb = small("hyb", W["l1_hy_bias"], [128, 4], stack=P3)
        Fv = Fm_d.rearrange("(c p) n -> p c n", p=128)
        for p in range(4):
            fs, fvw = ring.load(Fv[:, :, p * 512:(p + 1) * 512], 8, 512, q=sp)
            for q in range(4):
                bf_, bb_ = K.bank(), K.bank()
                for part, bk in ((0, bf_), (1, bb_)):
                    for pc in range(8):
                        h_ap, h_t = HFB(part, pc)
                        K.mm(bk, bk.t[:], fvw[:, pc, q * 128:(q + 1) * 128], h_ap, r=(fs, h_t), start=(pc == 0), stop=(pc == 7))
                hbs = rotbuf("hbs", 1, [128, 512], F32, P3)
                K.op(act, lambda h, hbs=hbs, bb_=bb_: h.activation(out=hbs.t[:], in_=bb_.t[:], func=AF.Copy), r=(bb_,), w=(hbs,))
                K.op(dve, lambda h, q=q, hbs=hbs, bf_=bf_: h.tensor_tensor(out=Kt[q].t[:], in0=bf_.t[:], in1=hbs.t[:], op=(ALU.add if q % 2 == 0 else ALU.subtract)),
                     r=(bf_, hbs), w=(Kt[q],))
                if p == 0 and q == 1:
                    K.op(dve, lambda h, hbs=hbs, bf_=bf_: h.tensor_tensor(out=Kt[1].t[0:1, :], in0=bf_.t[0:1, :], in1=hbs.t[0:1, :], op=ALU.add),
                         r=(bf_, hbs, Kt[1]), w=(Kt[1],))
                K.op(dve, lambda h, q=q: h.tensor_tensor(out=Kt[q].t[:], in0=Kt[q].t[:], in1=rS.t[:], op=ALU.mult), r=(Kt[q], rS), w=(Kt[q],))
            for j in range(4):
                zb = []
                for q in range(4):
                    bk = K.bank()
                    for i2, tc in enumerate((2 * j, 2 * j + 1)):
                        z_ap, z_t = ZTM(tc)
                        K.mm(bk, bk.t[:], fvw[:, tc, q * 128:(q + 1) * 128], z_ap, r=(fs, z_t), start=(i2 == 0), stop=(i2 == 1))
                    zs = rotbuf("zs", 7, [128, 512], F32, P3)
                    K.op(act, lambda h, zs=zs, bk=bk: h.activation(out=zs.t[:], in_=bk.t[:], func=AF.Copy), r=(bk,), w=(zs,))
                    zb.append(zs)
                for cc in range(2):
                    zre, zim, kre, kim = zb[2 * cc], zb[2 * cc + 1], Kt[2 * cc], Kt[2 * cc + 1]
                    t1 = rotbuf("sp1", 2, [128, 512], F32, P3)
                    t2 = rotbuf("sp2", 2, [128, 512], F32, P3)
                    K.op(dve, lambda h, t1=t1, kre=kre, zre=zre: h.tensor_tensor(out=t1.t[:], in0=kre.t[:], in1=zre.t[:], op=ALU.mult), r=(kre, zre), w=(t1,))
                    K.op(dve, lambda h, t2=t2, kim=kim, zim=zim: h.tensor_tensor(out=t2.t[:], in0=kim.t[:], in1=zim.t[:], op=ALU.mult), r=(kim, zim), w=(t2,))
                    yre, yim = Ys[j][2 * cc], Ys[j][2 * cc + 1]
                    K.op(dve, lambda h, t1=t1, t2=t2, yre=yre: h.tensor_tensor(out=yre.t[:], in0=t1.t[:], in1=t2.t[:], op=ALU.subtract), r=(t1, t2), w=(yre,))
                    if p == 0 and cc == 0:
                        K.op(dve, lambda h, t1=t1, yre=yre: h.tensor_copy(out=yre.t[0:1, :], in_=t1.t[0:1, :]), r=(t1, yre), w=(yre,))
                    t3 = rotbuf("sp1", 2, [128, 512], F32, P3)
                    t4 = rotbuf("sp2", 2, [128, 512], F32, P3)
                    K.op(dve, lambda h, t3=t3, kre=kre, zim=zim: h.tensor_tensor(out=t3.t[:], in0=kre.t[:], in1=zim.t[:], op=ALU.mult), r=(kre, zim), w=(t3,))
                    K.op(dve, lambda h, t4=t4, kim=kim, zre=zre: h.tensor_tensor(out=t4.t[:], in0=kim.t[:], in1=zre.t[:], op=ALU.mult), r=(kim, zre), w=(t4,))
                    K.op(dve, lambda h, t3=t3, t4=t4, yim=yim: h.tensor_tensor(out=yim.t[:], in0=t3.t[:], in1=t4.t[:], op=ALU.add), r=(t3, t4), w=(yim,))
                    if p == 0 and cc == 0:
                        K.op(dve, lambda h, t2=t2, yim=yim: h.tensor_copy(out=yim.t[0:1, :], in_=t2.t[0:1, :]), r=(t2, yim), w=(yim,))
            ifs = []
            for j in range(4):
                ifs.append(ring.load(iFm_d[j, p * 512:(p + 1) * 512, :].rearrange("(q f) t -> f q t", f=128), 4, T, q=sp))
            for jo in range(4):
                for hh in range(2):
                    bk = K.bank()
                    n = 0
                    for j in range(4):
                        for q in range(4):
                            K.mm(bk, bk.t[:], Ys[j][q].t[:, jo * 128:(jo + 1) * 128], ifs[j][1][:, q, hh * 512:(hh + 1) * 512], r=(Ys[j][q], ifs[j][0]),
                                 start=(n == 0), stop=(n == 15))
                            n += 1
                    if p == 0:
                        K.op(act, lambda h, jo=jo, hh=hh, bk=bk: h.activation(out=yacc[jo].t[:, HS[hh]], in_=bk.t[:], func=AF.Copy), r=(bk,), w=(yacc[jo],))
                    else:
                        K.op(dve, lambda h, jo=jo, hh=hh, bk=bk: h.tensor_tensor(out=yacc[jo].t[:, HS[hh]], in0=bk.t[:], in1=yacc[jo].t[:, HS[hh]], op=ALU.add),
                             r=(bk, yacc[jo]), w=(yacc[jo],))
        for jo in range(4):
            K.op(dve, lambda h, jo=jo: h.scalar_tensor_tensor(out=yacc[jo].t[:], in0=hs.t[:, jo, :], scalar=hyb.t[:, jo:jo + 1], op0=ALU.mult,
                                                           in1=yacc[jo].t[:], op1=ALU.add), r=(hh_[jo][0], hh_[jo][1], hyb, yacc[jo]), w=(yacc[jo],))
            K.op(dve, lambda h, jo=jo: h.tensor_tensor(out=ym[8 + jo].t[:], in0=ym[8 + jo].t[:], in1=yacc[jo].t[:], op=ALU.mult),
                 r=(ym[8 + jo], yacc[jo]), w=(ym[8 + jo],))
        K.barrier()
        P3.close()

        P4 = contextlib.ExitStack()
        rcos = small("rcos", ropecos_d, [64, T], stack=P4)
        rsin = small("rsin", ropesin_d, [64, T], stack=P4)
        qr_bufs = [K.sb(f"qr{i}", [128, T], BF16, P4) for i in range(2)]
        for qb in qr_bufs:
            K.op(pool, lambda h, qb=qb: h.memset(qb.t[:], 0.0), w=(qb,))
            K.dma(sp, qb.t[64:68, :], mk_r_d, qb, r=(qb,), w=(qb,))
        vt = [K.sb(f"v{kt}", [128, 1024], BF16, P4) for kt in range(NKT)]
        wqn = ring.load(wview(W["l1_wq_nope"], 0, 3, 0, 1024), 3, 1024)
        wqr = ring.load(wview(W["l1_wq_rope"], 0, 3, 0, 512), 3, 512)
        wqs = ring.load(wview(W["l1_wq_rope_sw"], 0, 3, 0, 512), 3, 512)
        wkn = ring.load(wview(W["l1_wk_nope"], 0, 2, 0, 1024), 2, 1024)
        wvv = ring.load(wview(W["l1_wv"], 0, 2, 0, 1024), 2, 1024)
        for kt in range(NKT):
            for c2 in range(2):
                bk = K.bank()
                for kc in range(2):
                    K.mm(bk, bk.t[:], ckv_all[kc].t[:, kt * 128:(kt + 1) * 128], wvv[1][:, kc, c2 * 512:(c2 + 1) * 512], r=(ckv_all[kc], wvv[0]),
                         start=(kc == 0), stop=(kc == 1))
                K.op(act, lambda h, kt=kt, c2=c2, bk=bk: h.activation(out=vt[kt].t[:, c2 * 512:(c2 + 1) * 512], in_=bk.t[:], func=AF.Copy), r=(bk,), w=(vt[kt],))
        def head_proj(hd_):
            qn = rotbuf("qn", 2, [128, T], BF16, P4)
            qr = qr_bufs[hd_ % 2]
            kn = rotbuf("kn", 2, [128, LK], BF16, P4)
            bq = proj(wqn[1], wqn[0], hd_ * 128, rhs=lambda kc, hh: cqn[kc].t[:, HS[hh]], rt=lambda kc, hh: cqn[kc], kcs=3)
            for hh in range(2):
                K.op(act, lambda h, hh=hh, qn=qn: h.activation(out=qn.t[:, HS[hh]], in_=bq[hh].t[:], func=AF.Copy), r=(bq[hh],), w=(qn,))
            br = proj(wqr[1], wqr[0], hd_ * 64, M=64, rhs=lambda kc, hh: cqn[kc].t[:, HS[hh]], rt=lambda kc, hh: cqn[kc], kcs=3)
            bs = proj(wqs[1], wqs[0], hd_ * 64, M=64, rhs=lambda kc, hh: cqn[kc].t[:, HS[hh]], rt=lambda kc, hh: cqn[kc], kcs=3)
            for hh in range(2):
                t1 = rotbuf("rt1", 2, [64, 512], F32, P4)
                t2 = rotbuf("rt2", 2, [64, 512], F32, P4)
                K.op(dve, lambda h, hh=hh, t1=t1: h.tensor_tensor(out=t1.t[:], in0=br[hh].t[0:64, :], in1=rcos.t[:, HS[hh]], op=ALU.mult), r=(br[hh], rcos), w=(t1,))
                K.op(dve, lambda h, hh=hh, t2=t2: h.tensor_tensor(out=t2.t[:], in0=bs[hh].t[0:64, :], in1=rsin.t[:, HS[hh]], op=ALU.mult), r=(bs[hh], rsin), w=(t2,))
                K.op(dve, lambda h, hh=hh, t1=t1, t2=t2, qr=qr: h.tensor_tensor(out=qr.t[0:64, HS[hh]], in0=t1.t[:], in1=t2.t[:], op=ALU.add), r=(t1, t2), w=(qr,))
            for g3 in range(3):
                bk = K.bank()
                for kc in range(2):
                    K.mm(bk, bk.t[:], wkn[1][:, kc, hd_ * 128:(hd_ + 1) * 128], ckv_all[kc].t[:, g3 * 512:(g3 + 1) * 512], r=(wkn[0], ckv_all[kc]),
                         start=(kc == 0), stop=(kc == 1))
                K.op(act, lambda h, g3=g3, bk=bk, kn=kn: h.activation(out=kn.t[:, g3 * 512:(g3 + 1) * 512], in_=bk.t[:], func=AF.Copy), r=(bk,), w=(kn,))
            return qn, qr, kn

        nxt = head_proj(0)
        for hd_ in range(8):
            qn, qr, kn = nxt
            for hh in range(2):
                ob = K.banks[0 + 2 * (hh % 2)]
                db = K.banks[1 + 2 * (hh % 2)]

                def s_mm(kt, hh=hh):
                    sbk = K.banks[4 + (kt % 4)]
                    K.mm(sbk, sbk.t[:], kn.t[:, kt * 128:(kt + 1) * 128], qn.t[:, HS[hh]], r=(kn, qn), start=True, stop=False)
                    K.mm(sbk, sbk.t[:], kr_all.t[:, kt * 128:(kt + 1) * 128], qr.t[:, HS[hh]], r=(kr_all, qr), start=False, stop=True)
                s_mm(0)
                s_mm(1)
                s_mm(2)
                for kt in range(NKT):
                    sbk = K.banks[4 + (kt % 4)]
                    pT = rotbuf("pT", 4, [128, 512], BF16, P4)
                    K.op(act, lambda h, sbk=sbk, pT=pT: h.activation(out=pT.t[:], in_=sbk.t[:], func=AF.Exp, scale=SC), r=(sbk,), w=(pT,))
                    if kt + 3 < NKT:
                        s_mm(kt + 3)
                    K.mm(ob, ob.t[:], vt[kt].t[:, hd_ * 128:(hd_ + 1) * 128], pT.t[:], r=(vt[kt], pT), start=(kt == 0), stop=(kt == NKT - 1), finc=True)
                    K.mm(db, db.t[:], ones_bf.t[:], pT.t[:], r=(ones_bf, pT), start=(kt == 0), stop=(kt == NKT - 1), finc=True)
                rec = rotbuf("rec", 2, [128, 512], F32, P4)
                K.op(dve, lambda h, rec=rec, db=db: h.reciprocal(out=rec.t[:], in_=db.t[:]), r=(db,), w=(rec,))
                K.op(dve, lambda h, hh=hh, rec=rec, ob=ob, hd_=hd_: h.tensor_tensor(out=ym[hd_].t[:, HS[hh]], in0=ob.t[:], in1=rec.t[:], op=ALU.mult),
                     r=(ob, rec), w=(ym[hd_],))
                if hh == 0 and hd_ + 1 < 8:
                    nxt = head_proj(hd_ + 1)
        K.barrier()
        P4.close()
        out_proj(W["l1_w_out"], lambda oc: mod1.t[:, 16 + oc:17 + oc], mod1)
        if debug:
            for c in range(2):
                dbg_dump(4 + c, xs.t[:, c, :], (xh[c][0], xh[c][1]))

        MO = contextlib.ExitStack()
        wr = K.sb("wr", [128, 8, 8], F32, MO)
        K.dma(sp, wr.t[:], W["l1_router_w"].rearrange("(c p) e -> p c e", p=128), wr, w=(wr,))
        rbias = small("rbias", W["l1_router_b"], [8, 1], stack=MO)
        sel8 = small("sel8", sel8_d, [8, 1024], dt=BF16, stack=MO)
        lg = K.sb("lg", [8, T], F32, MO)
        lt = K.sb("lt", [128, 64], F32, MO)
        gts = K.sb("gts", [128, 64], F32, MO)
        gT = K.sb("gT", [8, T], BF16, MO)
        rt = {"wr": wr, "banks": []}
        norm_mod(lambda c: gs2b.t[:, c:c + 1], lambda c: mod1.t[:, 24 + c:25 + c], (gs2b, mod1), router=rt)
        for hh in range(2):
            rb = rt["banks"][hh]
            K.op(act, lambda h, hh=hh, rb=rb: h.activation(out=lg.t[:, HS[hh]], in_=rb.t[0:8, :], func=AF.Identity, bias=rbias.t[:, 0:1], scale=1.0),
                 r=(rb, rbias), w=(lg,))
        ltb = K.bank()
        for tt in range(8):
            K.transpose(ltb, ltb.t[:, tt * 8:(tt + 1) * 8], lg.t[0:8, tt * 128:(tt + 1) * 128], ident.t[0:8, 0:8], r=(lg, ident), first=(tt == 0), last=(tt == 7))
        K.op(dve, lambda h: h.tensor_copy(out=lt.t[:], in_=ltb.t[:, 0:64]), r=(ltb,), w=(lt,))
        sm_ = [K.sb(f"rs{i}", [128, 8], F32, MO) for i in range(4)]
        sc_ = [K.sb(f"rc{i}", [128, 1], F32, MO) for i in range(4)]
        for tt in range(8):
            l_ap = lt.t[:, tt * 8:(tt + 1) * 8]
            m1, nm1, m2, ssum = sc_
            ex, eq, l2, sel = sm_
            K.op(dve, lambda h: h.reduce_max(out=m1.t[:], in_=l_ap, axis=mybir.AxisListType.X), r=(lt,), w=(m1,))
            K.op(dve, lambda h: h.tensor_scalar(out=nm1.t[:], in0=m1.t[:], scalar1=-1.0, scalar2=None, op0=ALU.mult), r=(m1,), w=(nm1,))
            K.op(act, lambda h: h.activation(out=ex.t[:], in_=l_ap, func=AF.Exp, bias=nm1.t[:, 0:1], scale=1.0), r=(lt, nm1), w=(ex,))
            K.op(dve, lambda h: h.tensor_scalar(out=eq.t[:], in0=l_ap, scalar1=m1.t[:, 0:1], scalar2=None, op0=ALU.is_equal), r=(lt, m1), w=(eq,))
            K.op(dve, lambda h: h.scalar_tensor_tensor(out=l2.t[:], in0=eq.t[:], scalar=-1e30, op0=ALU.mult, in1=l_ap, op1=ALU.add), r=(eq, lt), w=(l2,))
            K.op(dve, lambda h: h.reduce_max(out=m2.t[:], in_=l2.t[:], axis=mybir.AxisListType.X), r=(l2,), w=(m2,))
            K.op(dve, lambda h: h.tensor_scalar(out=sel.t[:], in0=l_ap, scalar1=m2.t[:, 0:1], scalar2=None, op0=ALU.is_ge), r=(lt, m2), w=(sel,))
            K.op(dve, lambda h: h.tensor_tensor(out=ex.t[:], in0=ex.t[:], in1=sel.t[:], op=ALU.mult), r=(ex, sel), w=(ex,))
            K.op(dve, lambda h: h.reduce_sum(out=ssum.t[:], in_=ex.t[:], axis=mybir.AxisListType.X), r=(ex,), w=(ssum,))
            K.op(dve, lambda h: h.reciprocal(out=ssum.t[:], in_=ssum.t[:]), r=(ssum,), w=(ssum,))
            K.op(dve, lambda h, tt=tt: h.tensor_scalar(out=gts.t[:, tt * 8:(tt + 1) * 8], in0=ex.t[:], scalar1=ssum.t[:, 0:1], scalar2=None, op0=ALU.mult),
                 r=(ex, ssum), w=(gts,))
        for g2 in range(2):
            gb = K.bank()
            for t4 in range(4):
                tt = g2 * 4 + t4
                K.transpose(gb, gb.t[0:8, t4 * 128:(t4 + 1) * 128], gts.t[:, tt * 8:(tt + 1) * 8], ident.t[:], r=(gts, ident), first=(t4 == 0), last=(t4 == 3))
            K.op(act, lambda h, g2=g2, gb=gb: h.activation(out=gT.t[:, HS[g2]], in_=gb.t[0:8, :], func=AF.Copy), r=(gb,), w=(gT,))
        for e in range(8 if stage >= 7 else 0):
            bg = rotbuf("bg", 2, [128, T], BF16, MO)
            for hh in range(2):
                bk = K.bank()
                K.mm(bk, bk.t[:], sel8.t[:, e * 128:(e + 1) * 128], gT.t[:, HS[hh]], r=(sel8, gT), start=True, stop=True)
                K.op(act, lambda h, hh=hh, bk=bk, bg=bg: h.activation(out=bg.t[:, HS[hh]], in_=bk.t[:], func=AF.Copy), r=(bk,), w=(bg,))
            ffn_unit(W["l1_exp_gate"][e], W["l1_exp_up"][e], W["l1_exp_down"][e], 0, 0,
                     lambda oc: mod1.t[:, 40 + oc:41 + oc], mod1, MO, bg=bg)
        K.barrier()
        MO.close()
        K.barrier()
        L1.close()

    FN = contextlib.ExitStack()
    yv = y_d.rearrange("(c p) t -> p c t", p=128)
    yo4 = [K.sb(f"yq{i}", [128, 512], F32, FN) for i in range(4)]
    cnt = [0]
    def fin_half(hh):
        bank = K.bank()
        for c in range(8):
            sq = rotbuf("sq", 3, [128, 512], BF16, FN)
            K.op(act, lambda h, c=c, sq=sq: h.activation(out=sq.t[:], in_=X(c, hh), func=AF.Square), r=(xh[c][hh],), w=(sq,))
            yield
            K.mm(bank, bank.t[:], ones_bf.t[:], sq.t[:], r=(sq, ones_bf), start=(c == 0), stop=(c == 7), finc=True)
            yield
        rstd = rotbuf("rstd", 2, [128, 512], F32, FN)
        K.op(act, lambda h: h.activation(out=rstd.t[:], in_=bank.t[:], func=AF.Ln, bias=EPS, scale=1.0 / D), r=(bank,), w=(rstd,))
        yield
        K.op(act, lambda h: h.activation(out=rstd.t[:], in_=rstd.t[:], func=AF.Exp, scale=-0.5), r=(rstd,), w=(rstd,))
        yield
        for c in range(8):
            t = yo4[cnt[0] % 4]
            cnt[0] += 1
            K.op(dve, lambda h, c=c, t=t: h.scalar_tensor_tensor(out=t.t[:], in0=X(c, hh), scalar=fin_g.t[:, c:c + 1], op0=ALU.mult,
                                                                in1=rstd.t[:], op1=ALU.mult),
                 r=(xh[c][hh], rstd, fin_g), w=(t,))
            yield
            K.dma(sp, yv[:, c, hh * 512:(hh + 1) * 512], t.t[:], t, r=(t,))
            yield
    zipper(fin_half(0), fin_half(1))
    K.barrier()
    FN.close()
    K.finish()
    return nc


def _fm(v, nch):
    return np.ascontiguousarray(np.asarray(v, np.float32).reshape(nch, 128).T)


_CST = {}


def _hy_tables(L):
    t = np.linspace(0.0, 1.0, L, dtype=np.float32)[:, None]
    w = (2.0 * math.pi * np.arange(L, dtype=np.float32)[:, None] / L).astype(np.float32)
    fb = np.linspace(1e-4, 16 - 1, 16, dtype=np.float32)[None, :]
    z = np.concatenate([t, np.cos(fb * w), -np.sin(fb * w)], axis=-1).astype(np.float32)
    max_decay = math.log(1e-2) / 0.3
    min_decay = math.log(1e-2) / 1.5
    deltas = np.linspace(min_decay, max_decay, 512, dtype=np.float32)
    decay = np.exp(-t * np.abs(deltas)).astype(np.float32)
    zin = np.zeros((33, T), np.float32)
    zin[:, :L] = z.T
    dec = np.zeros((T, 512), np.float32)
    dec[:L] = decay
    return zin, dec


def _constants():
    if _CST:
        return _CST
    bf = ml_dtypes.bfloat16
    sel = np.zeros((8, 8, 128), np.float32)
    for e in range(8):
        sel[e, e, :] = 1.0
    _CST["sel8"] = sel.reshape(8, 1024).astype(bf)
    rows = T // 64
    row = np.repeat(np.arange(rows, dtype=np.float32), 64)
    col = np.tile(np.arange(64, dtype=np.float32), rows)
    inv = (10000.0 ** (-np.arange(16, dtype=np.float32) / 16)).astype(np.float32)
    ang = np.concatenate([row[:, None] * inv, col[:, None] * inv], axis=-1).astype(np.float32)
    cos, sin = np.cos(ang).T, np.sin(ang).T
    _CST["ropecos_s"] = np.ascontiguousarray(np.concatenate([cos, cos], 0).astype(np.float32))
    _CST["ropesin_s"] = np.ascontiguousarray(np.concatenate([-sin, sin], 0).astype(np.float32))
    _CST["ropecos_p"] = np.ones((64, T), np.float32)
    _CST["ropesin_p"] = np.zeros((64, T), np.float32)
    _CST["maskb_s"] = np.zeros((128, NKT * 4), np.float32)
    mp = np.full((NKT, 4), NEG, np.float32)
    for kt in range(4, NKT):
        mp[kt, (kt - 4) // 2] = 0.0
    _CST["maskb_p"] = np.ascontiguousarray(np.broadcast_to(mp.reshape(1, NKT * 4), (128, NKT * 4)).astype(np.float32))
    mr = np.zeros((4, T), np.float32)
    for sg in range(4):
        mr[sg, sg * SEG:(sg + 1) * SEG] = 1.0
    _CST["mk_r"] = mr.astype(bf)
    _CST["mk_l_p"] = np.ascontiguousarray(np.repeat(mp.T[:, :, None], 128, axis=2).reshape(4, NKT * 128)).astype(bf)
    _CST["mk_l_s"] = np.zeros((4, NKT * 128), bf)
    _CST["zin_s"], _CST["decay_s"] = _hy_tables(T)
    _CST["zin_p"], _CST["decay_p"] = _hy_tables(SEG)
    N2 = 2 * T
    tt = np.arange(T, dtype=np.float64)[:, None]
    ff = np.arange(T, dtype=np.float64)[None, :]
    ang2 = 2.0 * np.pi * tt * ff / N2
    Fre = np.cos(ang2)
    Fim = -np.sin(ang2)
    Fim[:, 0] = np.cos(np.pi * tt[:, 0])
    Fm = np.zeros((T, N2), np.float64)
    iF = np.zeros((N2, T), np.float64)
    wre = np.full(T, 2.0 / N2)
    wre[0] = 1.0 / N2
    iFre = (np.cos(ang2) * wre[None, :]).T
    iFim = (-np.sin(ang2) * (2.0 / N2)).T
    iFim[0, :] = np.cos(np.pi * tt[:, 0]) / N2
    for c in range(8):
        Fm[:, (2 * c) * 128:(2 * c + 1) * 128] = Fre[:, c * 128:(c + 1) * 128]
        Fm[:, (2 * c + 1) * 128:(2 * c + 2) * 128] = Fim[:, c * 128:(c + 1) * 128]
        iF[(2 * c) * 128:(2 * c + 1) * 128] = iFre[c * 128:(c + 1) * 128]
        iF[(2 * c + 1) * 128:(2 * c + 2) * 128] = iFim[c * 128:(c + 1) * 128]
    _CST["Fm"] = Fm.astype(np.float32).astype(bf)
    iFb = iF.astype(np.float32).astype(bf)
    _CST["iFm_s"] = np.ascontiguousarray(np.broadcast_to(iFb[None], (4, N2, T)))
    ip = np.zeros((4, N2, T), bf)
    for j in range(4):
        ip[j, :, j * SEG:(j + 1) * SEG] = iFb[:, j * SEG:(j + 1) * SEG]
    _CST["iFm_p"] = ip
    return _CST


def prep_inputs(inp):
    f = lambda k: np.asarray(inp[k], np.float32)
    shared = {}
    for L in (0, 1):
        shared[f"l{L}_norm1"] = _fm(f(f"l{L}_norm1"), 8)
        shared[f"l{L}_norm2"] = _fm(f(f"l{L}_norm2"), 8)
        shared[f"l{L}_w_mod"] = f(f"l{L}_w_mod")
        shared[f"l{L}_b_mod"] = _fm(f(f"l{L}_b_mod"), 48)
    shared["l0_w_in"] = f("l0_w_in")
    shared["l0_conv_a"] = np.ascontiguousarray(f("l0_conv_a").reshape(3, 4, 128).transpose(2, 0, 1).reshape(128, 12))
    shared["l0_lru_conv_w"] = np.ascontiguousarray(f("l0_lru_conv_w").reshape(4, 8, 128).transpose(2, 0, 1).reshape(128, 32))
    shared["l0_lru_conv_b"] = _fm(f("l0_lru_conv_b"), 8)
    shared["l0_lru_wa"] = np.ascontiguousarray(f("l0_lru_wa").reshape(16 * 128, 128))
    shared["l0_lru_wi"] = np.ascontiguousarray(f("l0_lru_wi").reshape(16 * 128, 128))
    for k in ("l0_lru_ba", "l0_lru_bi", "l0_lru_lambda"):
        shared[k] = np.ascontiguousarray(f(k).reshape(2, 8, 128).transpose(2, 0, 1).reshape(128, 16))
    shared["l0_w_out"] = f("l0_w_out")
    shared["l0_ffn_gate"] = f("l0_ffn_gate")
    shared["l0_ffn_up"] = f("l0_ffn_up")
    shared["l0_ffn_down"] = f("l0_ffn_down")
    shared["final_norm"] = _fm(f("final_norm"), 8)
    w_in1 = f("l1_w_in")
    shared["l1_w_in"] = w_in1
    shared["l1_w_in_krsw"] = np.ascontiguousarray(np.concatenate([w_in1[:, 672:704], w_in1[:, 640:672]], axis=1))
    shared["l1_q_norm"] = _fm(f("l1_q_norm"), 3)
    shared["l1_kv_norm"] = _fm(f("l1_kv_norm"), 2)
    wq = f("l1_w_q_up").reshape(384, 8, 192)
    shared["l1_wq_nope"] = np.ascontiguousarray(wq[:, :, 0:128].reshape(384, 1024))
    shared["l1_wq_rope"] = np.ascontiguousarray(wq[:, :, 128:192].reshape(384, 512))
    shared["l1_wq_rope_sw"] = np.ascontiguousarray(np.concatenate([wq[:, :, 160:192], wq[:, :, 128:160]], axis=2).reshape(384, 512))
    wkv = f("l1_w_kv_up").reshape(256, 8, 256)
    shared["l1_wk_nope"] = np.ascontiguousarray(wkv[:, :, 0:128].reshape(256, 1024))
    shared["l1_wv"] = np.ascontiguousarray(wkv[:, :, 128:256].reshape(256, 1024))
    shared["l1_hy_short_w"] = np.ascontiguousarray(f("l1_hy_short_w").reshape(3, 12, 128).transpose(2, 0, 1).reshape(128, 36))
    shared["l1_hy_short_b"] = _fm(f("l1_hy_short_b"), 12)
    shared["l1_hy_f_w1"] = f("l1_hy_f_w1")
    shared["l1_hy_f_b1"] = f("l1_hy_f_b1").reshape(64, 1)
    shared["l1_hy_f_w2"] = f("l1_hy_f_w2")
    shared["l1_hy_f_b2"] = f("l1_hy_f_b2").reshape(64, 1)
    shared["l1_hy_f_w3"] = f("l1_hy_f_w3")
    shared["l1_hy_bias"] = _fm(f("l1_hy_bias"), 4)
    shared["l1_w_out"] = f("l1_w_out")
    shared["l1_router_w"] = f("l1_router_w")
    shared["l1_router_b"] = f("l1_router_b").reshape(8, 1)
    shared["l1_exp_gate"] = f("l1_exp_gate")
    shared["l1_exp_up"] = f("l1_exp_up")
    shared["l1_exp_down"] = f("l1_exp_down")
    cst = _constants()
    shared["sel8"] = cst["sel8"]
    shared["mk_r"] = cst["mk_r"]
    shared["Fm"] = cst["Fm"]

    xp, xsm = f("x_prompt"), f("x_sample")
    maps = []
    for core in range(NCORES):
        m = dict(shared)
        if core in (4, 5):
            b = core - 4
            m["xT"] = np.ascontiguousarray(xsm[b].T)
            m["cond"] = _fm(f("c")[b], 8)
            m["h0"] = np.ascontiguousarray(f("state_l0_lru")[b].reshape(2, 8, 128).transpose(2, 0, 1).reshape(128, 16))
            m["ctx_ckvT"] = np.ascontiguousarray(f("cache_l1_ckv")[b].T)
            m["ctx_krT"] = np.ascontiguousarray(f("cache_l1_krope")[b].T)
            for k in ("ropecos", "ropesin", "mk_l", "zin", "decay", "iFm"):
                m[k] = cst[k + "_s"]
            pf = 0.0
        else:
            pc = core if core < 4 else 0
            m["xT"] = np.ascontiguousarray(xp[4 * pc:4 * pc + 4].reshape(T, D).T)
            m["cond"] = _fm(f("c_ctx"), 8)
            m["h0"] = np.zeros((128, 16), np.float32)
            m["ctx_ckvT"] = np.zeros((256, LCTX), np.float32)
            m["ctx_krT"] = np.zeros((64, LCTX), np.float32)
            for k in ("ropecos", "ropesin", "mk_l", "zin", "decay", "iFm"):
                m[k] = cst[k + "_p"]
            pf = 1.0
        fl = np.zeros((128, 4), np.float32)
        fl[:, 0] = pf
        fl[:, 1] = -pf
        fl[:, 2] = 1.0 - pf
        m["flags"] = fl
        maps.append(m)
    return maps


_NC_CACHE = {}


def kernel(**inputs):
    if "nc" not in _NC_CACHE:
        _NC_CACHE["nc"] = build()
    nc = _NC_CACHE["nc"]
    maps = prep_inputs(inputs)
    res = run_bass_kernel_spmd(nc, maps, core_ids=list(range(NCORES)))
    R = res.results
    y_prompt = np.zeros((16, 256, D), np.float32)
    y_sample = np.zeros((2, T, D), np.float32)
    new_lru = np.zeros((16, 2, 1024), np.float32)
    new_ckv = np.zeros((16, 256, 256), np.float32)
    new_kr = np.zeros((16, 256, 64), np.float32)
    for core in range(4):
        new_ckv[4 * core:4 * core + 4] = R[core]["ckv_out"].T.reshape(4, 256, 256)
        new_kr[4 * core:4 * core + 4] = R[core]["kr_out"].T.reshape(4, 256, 64)
        yT = R[core]["yT"]
        y_prompt[4 * core:4 * core + 4] = yT.T.reshape(4, 256, D)
        lo = R[core]["lru_out"].reshape(128, 2, 8, 4)
        new_lru[4 * core:4 * core + 4] = lo.transpose(3, 1, 2, 0).reshape(4, 2, 1024)
    for b in range(2):
        y_sample[b] = R[4 + b]["yT"].T
    return y_prompt, y_sample, new_lru, new_ckv, new_kr
```
